# Optimizing a Trainium2 kernel written in Bass

```python
import math
import jax, jax.numpy as jnp
from jax import lax
import numpy as np

D_MODEL = 1024
BATCH = 32
SEQ = 2048
DEPTH = 1

CHUNK = 64
D_S5 = D_MODEL // 2
D_RWKV = D_MODEL - D_S5
S5_GROUP = 16
S5_GROUPS = D_S5 // S5_GROUP
S5_STATE = 64
RWKV_HEAD = 64
RWKV_HEADS = D_RWKV // RWKV_HEAD
W_RANK = 64
A_RANK = 64
G_RANK = 128
N_RW_COLS = 3 * D_RWKV + W_RANK + A_RANK + G_RANK
N_PROJ = D_S5 + N_RW_COLS
D_FF = 4 * D_MODEL
RMS_EPS = 1e-6
GN_EPS = 64e-5
DT_MIN = 1e-3
DT_MAX = 1e-1

kernel_name = "hymba_s5_rwkv7_sandwich_block"


def rmsnorm(x, g):
    xf = x.astype(jnp.float32)
    y = xf * lax.rsqrt(jnp.mean(xf * xf, axis=-1, keepdims=True) + RMS_EPS)
    return (y * g.astype(jnp.float32)).astype(x.dtype)


def _s5_combine(e1, e2):
    a1r, a1i, b1r, b1i = e1
    a2r, a2i, b2r, b2i = e2
    return (a1r * a2r - a1i * a2i,
            a1r * a2i + a1i * a2r,
            a2r * b1r - a2i * b1i + b2r,
            a2r * b1i + a2i * b1r + b2i)


def s5_mixer(u, lam_re, lam_im, log_dt, b_re, b_im, c_re, c_im, d_skip, w_glu, b_glu):
    f32 = jnp.float32
    Bsz, L, _ = u.shape
    uf = u.astype(f32).reshape(Bsz, L, S5_GROUPS, S5_GROUP)
    dt = jnp.exp(log_dt.astype(f32))[:, None]
    lr, li = lam_re.astype(f32), lam_im.astype(f32)
    mag = jnp.exp(lr * dt)
    ab_re = mag * jnp.cos(li * dt)
    ab_im = mag * jnp.sin(li * dt)
    den = lr * lr + li * li
    nr = ab_re - 1.0
    f_re = (nr * lr + ab_im * li) / den
    f_im = (ab_im * lr - nr * li) / den
    br, bi = b_re.astype(f32), b_im.astype(f32)
    bb_re = f_re[..., None] * br - f_im[..., None] * bi
    bb_im = f_re[..., None] * bi + f_im[..., None] * br
    bu_re = jnp.einsum('blgc,gpc->blgp', uf, bb_re)
    bu_im = jnp.einsum('blgc,gpc->blgp', uf, bb_im)
    a_re = jnp.broadcast_to(ab_re[None, None], (1, L, S5_GROUPS, S5_STATE))
    a_im = jnp.broadcast_to(ab_im[None, None], (1, L, S5_GROUPS, S5_STATE))
    _, _, s_re, s_im = lax.associative_scan(_s5_combine, (a_re, a_im, bu_re, bu_im), axis=1)
    y = (jnp.einsum('blgp,gcp->blgc', s_re, c_re.astype(f32))
         - jnp.einsum('blgp,gcp->blgc', s_im, c_im.astype(f32)))
    y = y.reshape(Bsz, L, D_S5) + d_skip.astype(f32) * uf.reshape(Bsz, L, D_S5)
    z = jax.nn.gelu(y)
    out = z * jax.nn.sigmoid(z @ w_glu.astype(f32) + b_glu.astype(f32))
    return out.astype(u.dtype)


def _rwkv7_recurrence(r, w, k, v, kk, a):
    Bsz, L, H, N = r.shape
    n_chunks = L // CHUNK

    def to_chunks(t):
        return jnp.moveaxis(t, 1, 0).reshape(n_chunks, CHUNK, Bsz, H, N)

    xs = (to_chunks(r), to_chunks(w), to_chunks(k), to_chunks(v), to_chunks(kk), to_chunks(a))

    def step(S, inp):
        r_t, w_t, k_t, v_t, kk_t, a_t = inp
        sa = jnp.einsum('bhvk,bhk->bhv', S, -kk_t)
        S = (S * w_t[:, :, None, :]
             + sa[..., None] * (kk_t * a_t)[:, :, None, :]
             + v_t[..., None] * k_t[:, :, None, :])
        return S, jnp.einsum('bhvk,bhk->bhv', S, r_t)

    def chunk_step(S, chunk):
        return lax.scan(step, S, chunk)

    S0 = jnp.zeros((Bsz, H, N, N), r.dtype)
    _, o = lax.scan(chunk_step, S0, xs)
    return jnp.moveaxis(o.reshape(L, Bsz, H, N), 0, 1)


def rwkv7_mixer(q, mu, w0, w_up, a0, a_up, g_up, k_k, k_a, r_k, gn_w, gn_b):
    f32 = jnp.float32
    Bsz, L, _ = q.shape
    H, N = RWKV_HEADS, RWKV_HEAD
    qf = q.astype(f32)
    prev = jnp.pad(qf, ((0, 0), (1, 0), (0, 0)))[:, :-1]
    qs = qf + mu.astype(f32) * (prev - qf)
    o1, o2, o3 = D_RWKV, 2 * D_RWKV, 3 * D_RWKV
    o4, o5 = o3 + W_RANK, o3 + W_RANK + A_RANK
    r, k, v = qs[..., :o1], qs[..., o1:o2], qs[..., o2:o3]
    xw, xa, xg = qs[..., o3:o4], qs[..., o4:o5], qs[..., o5:]
    w_log = -jax.nn.softplus(-(w0.astype(f32) + jnp.tanh(xw) @ w_up.astype(f32))) - 0.5
    decay = jnp.exp(-jnp.exp(w_log))
    a = jax.nn.sigmoid(a0.astype(f32) + xa @ a_up.astype(f32))
    g = jax.nn.sigmoid(xg) @ g_up.astype(f32)
    hd = lambda t: t.reshape(Bsz, L, H, N)
    kk = hd(k * k_k.astype(f32))
    kk = kk / jnp.maximum(jnp.linalg.norm(kk, axis=-1, keepdims=True), 1e-12)
    k = k * (1.0 + (a - 1.0) * k_a.astype(f32))
    rh, kh, vh, ah = hd(r), hd(k), hd(v), hd(a)
    o = _rwkv7_recurrence(rh, hd(decay), kh, vh, kk, ah)
    mean = jnp.mean(o, axis=-1, keepdims=True)
    var = jnp.mean(jnp.square(o - mean), axis=-1, keepdims=True)
    o = ((o - mean) * lax.rsqrt(var + GN_EPS)).reshape(Bsz, L, D_RWKV)
    o = o * gn_w.astype(f32) + gn_b.astype(f32)
    bonus = jnp.sum(rh * kh * r_k.astype(f32), axis=-1, keepdims=True) * vh
    out = (o + bonus.reshape(Bsz, L, D_RWKV)) * g
    return out.astype(q.dtype)


def hybrid_layer(x, g_pre_mix, w_in, s5_lam_re, s5_lam_im, s5_log_dt, s5_b_re, s5_b_im,
                 s5_c_re, s5_c_im, s5_d, s5_w_glu, s5_b_glu, rw_mu, rw_w0, rw_w_up, rw_a0,
                 rw_a_up, rw_g_up, rw_k_k, rw_k_a, rw_r_k, rw_gn_w, rw_gn_b, w_out,
                 g_post_mix, g_pre_mlp, w_ff_up, w_ff_down, g_post_mlp):
    xn = rmsnorm(x, g_pre_mix)
    p = xn @ w_in
    y_s5 = s5_mixer(p[..., :D_S5], s5_lam_re, s5_lam_im, s5_log_dt, s5_b_re, s5_b_im,
                    s5_c_re, s5_c_im, s5_d, s5_w_glu, s5_b_glu)
    y_rw = rwkv7_mixer(p[..., D_S5:], rw_mu, rw_w0, rw_w_up, rw_a0, rw_a_up, rw_g_up,
                       rw_k_k, rw_k_a, rw_r_k, rw_gn_w, rw_gn_b)
    mix = jnp.concatenate([y_s5, y_rw], axis=-1) @ w_out
    h = x + rmsnorm(mix, g_post_mix)
    hn = rmsnorm(h, g_pre_mlp)
    ff = jnp.square(jax.nn.relu(hn @ w_ff_up)) @ w_ff_down
    return h + rmsnorm(ff, g_post_mlp)


def setup_inputs(seed: int = 0) -> dict:
    key = jax.random.key(seed)
    ks = jax.random.split(key, 32)
    f32 = jnp.float32
    L = DEPTH

    def nrm(k, shape, scale):
        return jax.random.normal(k, shape, f32) * scale

    def gain(k, shape):
        return 1.0 + 0.02 * jax.random.normal(k, shape, f32)

    x = jax.random.normal(ks[0], (BATCH, SEQ, D_MODEL), f32)
    lam_im = (jnp.pi * jnp.arange(S5_STATE, dtype=f32))[None, None, :] \
        + 0.01 * jax.random.normal(ks[4], (L, S5_GROUPS, S5_STATE), f32)
    log_dt = jax.random.uniform(ks[5], (L, S5_GROUPS), f32,
                                math.log(DT_MIN), math.log(DT_MAX))
    return {
        "x": x,
        "g_pre_mix": gain(ks[1], (L, D_MODEL)),
        "w_in": nrm(ks[2], (L, D_MODEL, N_PROJ), D_MODEL ** -0.5),
        "s5_lam_re": -0.5 + 0.01 * jax.random.normal(ks[3], (L, S5_GROUPS, S5_STATE), f32),
        "s5_lam_im": lam_im,
        "s5_log_dt": log_dt,
        "s5_b_re": nrm(ks[6], (L, S5_GROUPS, S5_STATE, S5_GROUP), (2 * S5_GROUP) ** -0.5),
        "s5_b_im": nrm(ks[7], (L, S5_GROUPS, S5_STATE, S5_GROUP), (2 * S5_GROUP) ** -0.5),
        "s5_c_re": nrm(ks[8], (L, S5_GROUPS, S5_GROUP, S5_STATE), (2 * S5_STATE) ** -0.5),
        "s5_c_im": nrm(ks[9], (L, S5_GROUPS, S5_GROUP, S5_STATE), (2 * S5_STATE) ** -0.5),
        "s5_d": nrm(ks[10], (L, D_S5), 1.0),
        "s5_w_glu": nrm(ks[11], (L, D_S5, D_S5), D_S5 ** -0.5),
        "s5_b_glu": nrm(ks[12], (L, D_S5), 0.01),
        "rw_mu": jax.random.uniform(ks[13], (L, N_RW_COLS), f32),
        "rw_w0": nrm(ks[14], (L, D_RWKV), 0.5),
        "rw_w_up": nrm(ks[15], (L, W_RANK, D_RWKV), W_RANK ** -0.5),
        "rw_a0": nrm(ks[16], (L, D_RWKV), 0.1),
        "rw_a_up": nrm(ks[17], (L, A_RANK, D_RWKV), A_RANK ** -0.5),
        "rw_g_up": nrm(ks[18], (L, G_RANK, D_RWKV), G_RANK ** -0.5),
        "rw_k_k": 0.85 + 0.02 * jax.random.normal(ks[19], (L, D_RWKV), f32),
        "rw_k_a": gain(ks[20], (L, D_RWKV)),
        "rw_r_k": nrm(ks[21], (L, RWKV_HEADS, RWKV_HEAD), 0.1),
        "rw_gn_w": gain(ks[22], (L, D_RWKV)),
        "rw_gn_b": nrm(ks[23], (L, D_RWKV), 0.01),
        "w_out": nrm(ks[24], (L, D_MODEL, D_MODEL), D_MODEL ** -0.5),
        "g_post_mix": gain(ks[25], (L, D_MODEL)),
        "g_pre_mlp": gain(ks[26], (L, D_MODEL)),
        "w_ff_up": nrm(ks[27], (L, D_MODEL, D_FF), D_MODEL ** -0.5),
        "w_ff_down": nrm(ks[28], (L, D_FF, D_MODEL), D_FF ** -0.5),
        "g_post_mlp": gain(ks[29], (L, D_MODEL)),
    }


def reference(x, g_pre_mix, w_in, s5_lam_re, s5_lam_im, s5_log_dt, s5_b_re, s5_b_im,
              s5_c_re, s5_c_im, s5_d, s5_w_glu, s5_b_glu, rw_mu, rw_w0, rw_w_up, rw_a0,
              rw_a_up, rw_g_up, rw_k_k, rw_k_a, rw_r_k, rw_gn_w, rw_gn_b, w_out,
              g_post_mix, g_pre_mlp, w_ff_up, w_ff_down, g_post_mlp):
    h = x
    for l in range(DEPTH):
        h = hybrid_layer(h, g_pre_mix[l], w_in[l], s5_lam_re[l], s5_lam_im[l], s5_log_dt[l],
                         s5_b_re[l], s5_b_im[l], s5_c_re[l], s5_c_im[l], s5_d[l],
                         s5_w_glu[l], s5_b_glu[l], rw_mu[l], rw_w0[l], rw_w_up[l], rw_a0[l],
                         rw_a_up[l], rw_g_up[l], rw_k_k[l], rw_k_a[l], rw_r_k[l],
                         rw_gn_w[l], rw_gn_b[l], w_out[l], g_post_mix[l], g_pre_mlp[l],
                         w_ff_up[l], w_ff_down[l], g_post_mlp[l])
    return h
```

```python
import contextlib
import types
import numpy as np
import concourse.bass as bass
import concourse.mybir as mybir
from concourse.bass_utils import run_bass_kernel_spmd

F32 = mybir.dt.float32
BF16 = mybir.dt.bfloat16
AF = mybir.ActivationFunctionType
ALU = mybir.AluOpType


def _freeze(fn):
    if fn.__closure__ is None:
        return fn
    cells = []
    for c in fn.__closure__:
        try:
            cells.append(types.CellType(c.cell_contents))
        except ValueError:
            cells.append(c)
    g = types.FunctionType(fn.__code__, fn.__globals__, fn.__name__, fn.__defaults__, tuple(cells))
    g.__kwdefaults__ = fn.__kwdefaults__
    return g


class Buf:
    def __init__(self, fw, name, t):
        self.fw = fw
        self.name = name
        self.t = t
        self.last_w = None
        self.readers = []
        self.ld_sem = None
        self.ld_cnt = 0
        self.st_sem = None
        self.st_cnt = 0

    def __getitem__(self, k):
        return self.t[k]


class FW:
    ENG = ("pe", "act", "dve", "pool", "sp")

    def __init__(self, nc):
        self.nc = nc
        self.stack = contextlib.ExitStack()
        self.tstack = contextlib.ExitStack()
        self.eng_obj = {"pe": nc.tensor, "act": nc.scalar, "dve": nc.vector,
                        "pool": nc.gpsimd, "sp": nc.sync}
        self.sem = {}
        self.cnt = {e: 0 for e in self.ENG}
        self.prog = {e: [] for e in self.ENG}
        self.waited = {e: {} for e in self.ENG}
        self.final_tokens = []
        self.all_sems = []
        self.same_engine_sync = True
        self.nsem = 0

    def scope(self):
        return self.stack

    def tscope(self):
        self.tstack = contextlib.ExitStack()
        return self.tstack

    def new_sem(self, name):
        self.nsem += 1
        s = self.stack.enter_context(self.nc.semaphore(f"{name}_{self.nsem}"))
        self.all_sems.append(s)
        return s

    def init_sems(self):
        for e in self.ENG:
            if e not in self.sem:
                self.sem[e] = self.new_sem("s_" + e)

    NOZERO = ("PA", "PB", "Win", "Wup", "Wdn")

    def sb(self, name, shape, dtype):
        t = self.tstack.enter_context(self.nc.sbuf_tensor(name, list(shape), dtype))
        b = Buf(self, name, t)
        if name not in self.NOZERO:
            self.op("pool", lambda e: e.memset(b[:], 0.0), writes=[b])
        return b

    def ps(self, name, shape, dtype):
        t = self.tstack.enter_context(self.nc.psum_tensor(name, list(shape), dtype))
        b = Buf(self, name, t)
        self.op("dve", lambda e: e.memset(b[:], 0.0), writes=[b])
        return b

    def view(self, name, ap):
        return Buf(self, name, ap)

    def _deps(self, reads, writes):
        deps = []
        for b in reads:
            if b.last_w is not None:
                deps.append(b.last_w)
        for b in writes:
            if b.last_w is not None:
                deps.append(b.last_w)
            deps.extend(b.readers)
        return deps

    def _emit_waits(self, eng, deps):
        w = self.waited[eng]
        for (sem, val, src) in deps:
            if src == eng and (eng == "pe" or not self.same_engine_sync):
                continue
            key = id(sem)
            if w.get(key, 0) >= val:
                continue
            w[key] = val
            self.prog[eng].append(("wait", sem, val))

    def op(self, eng, fn, reads=(), writes=()):
        self.init_sems()
        reads = [b for b in reads if b is not None]
        writes = [b for b in writes if b is not None]
        self._emit_waits(eng, self._deps(reads, writes))
        self.cnt[eng] += 1
        tok = (self.sem[eng], self.cnt[eng], eng)
        self.prog[eng].append(("op", _freeze(fn), self.sem[eng], 1))
        for b in reads:
            b.readers.append(tok)
        for b in writes:
            b.last_w = tok
            b.readers = []
        return tok

    def dma(self, q, dst, out_ap, in_ap, reads=(), out_final=False, **kw):
        self.init_sems()
        reads = [b for b in reads if b is not None]
        writes = [dst] if dst is not None else []
        self._emit_waits(q, self._deps(reads, writes))
        if dst is not None:
            if dst.ld_sem is None:
                dst.ld_sem = self.new_sem("ld_" + dst.name)
            dst.ld_cnt += 16
            tok = (dst.ld_sem, dst.ld_cnt, "dma")
            sem = dst.ld_sem
            dst.last_w = tok
            dst.readers = []
            for b in reads:
                b.readers.append(tok)
        else:
            src = reads[0]
            if src.st_sem is None:
                src.st_sem = self.new_sem("st_" + src.name)
            src.st_cnt += 16
            tok = (src.st_sem, src.st_cnt, "dma")
            sem = src.st_sem
            for b in reads:
                b.readers.append(tok)
            if out_final:
                self.final_tokens.append(tok)
        fn = (lambda e, o=out_ap, i=in_ap, k=kw: e.dma_start(out=o, in_=i, **k))
        self.prog[q].append(("op", fn, sem, 16))
        return tok

    def drain_all(self):
        self.init_sems()
        deps = [(self.sem[e], self.cnt[e], e) for e in self.ENG if e != "sp" and self.cnt[e] > 0]
        deps += self.final_tokens
        self._emit_waits("sp", deps)

    def emit(self):
        self.drain_all()
        nc = self.nc
        with nc.Block() as block:
            def mk(eng):
                def body(e):
                    for item in self.prog[eng]:
                        if item[0] == "wait":
                            e.wait_ge(item[1], item[2])
                        else:
                            ins = item[1](e)
                            ins.then_inc(item[2], item[3])
                return body
            block.tensor(mk("pe"))
            block.scalar(mk("act"))
            block.vector(mk("dve"))
            block.gpsimd(mk("pool"))
            block.sync(mk("sp"))
        self.prog = {e: [] for e in self.ENG}


NCORES = 8
NB = 4
SEQ = 2048
D = 1024
TC = 64
NIT = SEQ // TC
NCOL = NB * TC
DS5 = 512
NPROJ = 2304
DFF = 4096
SUB = 16
GN_EPS = 64e-5


class PP:
    def __init__(self):
        self.off = {}
        self.n = 0
        self.parts = []

    def add(self, name, arr):
        arr = np.ascontiguousarray(arr, dtype=np.float32)
        if arr.shape[0] < 128:
            pad = np.zeros((128 - arr.shape[0],) + arr.shape[1:], np.float32)
            arr = np.concatenate([arr, pad], 0)
        arr = arr.reshape(128, -1)
        self.off[name] = (self.n, arr.shape[1])
        self.n += arr.shape[1]
        self.parts.append(arr)

    def pack(self):
        return np.ascontiguousarray(np.concatenate(self.parts, 1))


def pk(v, nt):
    return np.asarray(v, np.float32).reshape(nt, 128).T


def build_params(inp):
    g = lambda k: np.asarray(inp[k], np.float32)[0]
    A = PP()
    A.add("gpre", pk(g("g_pre_mix"), 8))
    s5l = lambda a: a.reshape(16, 2, 64).transpose(1, 2, 0).reshape(128, 16)
    A.add("lamre", s5l(g("s5_lam_re")))
    A.add("lamim", s5l(g("s5_lam_im")))
    A.add("logdt", s5l(np.repeat(g("s5_log_dt")[:, None], 64, 1)))
    s5b = lambda a: a.reshape(16, 2, 64, 16).transpose(1, 2, 0, 3).reshape(128, 16 * 16)
    A.add("bre", s5b(g("s5_b_re")))
    A.add("bim", s5b(g("s5_b_im")))

    def ctl(c):
        o = np.zeros((2, 64, 16, 2, 16), np.float32)
        cc = c.reshape(16, 2, 16, 64)
        for g2 in range(2):
            o[g2, :, :, g2, :] = cc[:, g2].transpose(2, 0, 1)
        return o.reshape(128, 16 * 32)
    A.add("ctre", ctl(g("s5_c_re")))
    A.add("ctim", ctl(g("s5_c_im")))
    def pk96(v):
        o = np.zeros((128, 6), np.float32)
        for tl in range(6):
            n = 96 if tl < 5 else 32
            o[:n, tl] = v[96 * tl:96 * tl + n]
        return o
    A.add("dskip", pk96(g("s5_d")))
    A.add("bglu", pk96(g("s5_b_glu")))
    A.add("mu", pk(g("rw_mu"), 14))
    A.add("w0", pk(g("rw_w0"), 4))
    A.add("a0", pk(g("rw_a0"), 4))
    A.add("kk", pk(g("rw_k_k"), 4))
    A.add("ka", pk(g("rw_k_a"), 4))
    A.add("rk", pk(g("rw_r_k").reshape(-1), 4))
    A.add("gnw", pk(g("rw_gn_w"), 4))
    A.add("gnb", pk(g("rw_gn_b"), 4))
    A.add("wau", np.concatenate([g("rw_w_up"), g("rw_a_up")], 0))
    A.add("gup", g("rw_g_up"))
    A.add("gBmix", np.repeat(g("g_post_mix")[None, :], 128, 0))
    A.add("ident", np.eye(128, dtype=np.float32))
    ob = np.zeros((128, 128), np.float32)
    ob[:64, :64] = 1
    ob[64:, 64:] = 1
    A.add("onesblk", ob)
    j = np.arange(64)[:, None]
    t = np.arange(64)[None, :]
    mg = np.concatenate([(t > j), (t >= j)], 1).astype(np.float32)
    A.add("maskG", np.concatenate([mg, mg], 0))
    A.add("maskL", np.concatenate([(j > t), (j > t)], 0).astype(np.float32))
    sm = np.ones((128, NB, TC), np.float32)
    sm[:, :, 0] = 0
    A.add("scanmask", sm.reshape(128, NCOL))
    B = PP()
    B.add("gpremlp", pk(g("g_pre_mlp"), 8))
    B.add("gBmlp", np.repeat(g("g_post_mlp")[None, :], 128, 0))
    B.add("ident", np.eye(128, dtype=np.float32))
    return A, B


def build_program(offA, nA, offB, nB, nit=NIT, do_ffn=True):
    nc = bass.Bass("TRN2", target_bir_lowering=False)
    x = nc.dram_tensor("x", [NB, SEQ, D], F32, kind="ExternalInput").ap()
    ppa = nc.dram_tensor("ppa", [128, nA], F32, kind="ExternalInput").ap()
    ppb = nc.dram_tensor("ppb", [128, nB], F32, kind="ExternalInput").ap()
    w_in = nc.dram_tensor("w_in", [D, NPROJ], F32, kind="ExternalInput").ap()
    w_out = nc.dram_tensor("w_out", [D, D], F32, kind="ExternalInput").ap()
    w_glu = nc.dram_tensor("w_glu", [DS5, DS5], F32, kind="ExternalInput").ap()
    w_up = nc.dram_tensor("w_ff_up", [D, DFF], F32, kind="ExternalInput").ap()
    w_dn = nc.dram_tensor("w_ff_down", [DFF, D], F32, kind="ExternalInput").ap()
    out = nc.dram_tensor("out", [NB, SEQ, D], F32, kind="ExternalOutput").ap()
    hsc = nc.dram_tensor("hsc", [NB, SEQ, D], F32, kind="ExternalOutput").ap()

    import os
    SKIP = os.environ.get("KSKIP", "")
    TAPS = set(os.environ.get("KTAPS", "").split(",")) - {""}
    KSTOP = int(os.environ.get("KSTOP", "99"))
    SCAN_ENG = os.environ.get("KSCAN", "pool")
    fw = FW(nc)
    tapped = set()

    def tap(name, buf, ap):
        if name not in TAPS or name in tapped:
            return
        tapped.add(name)
        dt_ = ap.dtype
        dd = nc.dram_tensor("dbg_" + name, list(ap.shape), dt_, kind="ExternalOutput").ap()
        fw.dma("sp", None, dd, ap, reads=[buf], out_final=True)
    fw.stack.__enter__()
    with fw.tscope():
        sb, op, dma = fw.sb, fw.op, fw.dma
        PA = sb("PA", [128, nA], F32)

        def P(name, c0=0, c1=None, rows=slice(0, 128)):
            o, n = offA[name]
            c1 = n if c1 is None else c1
            return PA[rows, o + c0:o + c1]
        Win = sb("Win", [128, 8, NPROJ], BF16)
        Wout = sb("Wout", [128, 10, D], BF16)
        Wglu = sb("Wglu", [128, 6, DS5], BF16)
        TR = [96, 96, 96, 96, 96, 32]
        banks = [fw.ps(f"bank{i}", [128, 512], F32) for i in range(8)]
        bi = [0]

        def bank():
            b = banks[bi[0] % 8]
            bi[0] += 1
            return b

        dma("sp", PA, PA[:], ppa)
        for k in range(8):
            for h in range(2):
                dma("pool", Win, Win[:, k, h * 1152:(h + 1) * 1152], w_in[k * 128:(k + 1) * 128, h * 1152:(h + 1) * 1152])
        for k in range(6):
            dma("pool", Wout, Wout[0:TR[k], k, :], w_out[k * 96:k * 96 + TR[k], :])
            dma("pool", Wglu, Wglu[0:TR[k], k, :], w_glu[k * 96:k * 96 + TR[k], :])
        for k in range(4):
            dma("pool", Wout, Wout[:, 6 + k, :], w_out[512 + k * 128:512 + (k + 1) * 128, :])

        s5t = sb("s5t", [128, 12, 16], F32)
        T = lambda i: s5t[:, i, :]
        AR, AI, FR, FI = 0, 1, 2, 3
        tt_ = lambda o, a, b, f: op("dve", lambda e: e.tensor_tensor(out=o, in0=a, in1=b, op=f), reads=[PA, s5t], writes=[s5t])
        ts_ = lambda o, a, s1, s2, f1, f2: op("dve", lambda e: e.tensor_scalar(out=o, in0=a, scalar1=s1, scalar2=s2, op0=f1, op1=f2), reads=[PA, s5t], writes=[s5t])
        ac_ = lambda o, a, f, **kw: op("act", lambda e: e.activation(out=o, in_=a, func=f, **kw), reads=[PA, s5t], writes=[s5t])
        ac_(T(4), P("logdt"), AF.Exp)
        tt_(T(5), P("lamre"), T(4), ALU.mult)
        tt_(T(6), P("lamim"), T(4), ALU.mult)
        ac_(T(7), T(5), AF.Exp)
        ts_(T(8), T(6), 1.0 / 16, None, ALU.mult, ALU.bypass)
        ts_(T(9), T(6), 1.0 / 16, float(np.pi / 2), ALU.mult, ALU.add)
        ac_(T(8), T(8), AF.Sin)
        ac_(T(9), T(9), AF.Sin)
        for _ in range(4):
            tt_(T(10), T(8), T(9), ALU.mult)
            tt_(T(11), T(8), T(8), ALU.mult)
            tt_(T(9), T(9), T(9), ALU.mult)
            tt_(T(9), T(9), T(11), ALU.subtract)
            ts_(T(8), T(10), 2.0, None, ALU.mult, ALU.bypass)
        tt_(T(AR), T(7), T(9), ALU.mult)
        tt_(T(AI), T(7), T(8), ALU.mult)
        tt_(T(4), P("lamre"), P("lamre"), ALU.mult)
        tt_(T(5), P("lamim"), P("lamim"), ALU.mult)
        tt_(T(4), T(4), T(5), ALU.add)
        op("dve", lambda e: e.reciprocal(out=T(4), in_=T(4)), reads=[s5t], writes=[s5t])
        ts_(T(5), T(AR), -1.0, None, ALU.add, ALU.bypass)
        tt_(T(6), T(5), P("lamre"), ALU.mult)
        tt_(T(7), T(AI), P("lamim"), ALU.mult)
        tt_(T(6), T(6), T(7), ALU.add)
        tt_(T(FR), T(6), T(4), ALU.mult)
        tt_(T(6), T(AI), P("lamre"), ALU.mult)
        tt_(T(7), T(5), P("lamim"), ALU.mult)
        tt_(T(6), T(6), T(7), ALU.subtract)
        tt_(T(FI), T(6), T(4), ALU.mult)
        Acf = sb("Acf", [128, 2, 2, 16], F32)
        for c in range(2):
            op("dve", lambda e, c=c: e.tensor_copy(out=Acf[:, 0, c, :], in_=T(AR)), reads=[s5t], writes=[Acf])
        op("dve", lambda e: e.tensor_copy(out=Acf[:, 1, 0, :], in_=T(AI)), reads=[s5t], writes=[Acf])
        op("dve", lambda e: e.tensor_scalar(out=Acf[:, 1, 1, :], in0=T(AI), scalar1=-1.0, scalar2=None, op0=ALU.mult), reads=[s5t], writes=[Acf])
        tap("Acf", Acf, Acf[:])
        tap("s5t", s5t, s5t[:])
        BD = sb("BD", [128, 2, 96], F32)
        BT = sb("BT", [128, 6, 2, 128], BF16)
        bb1 = sb("bb1", [128, 16], F32)
        bb2 = sb("bb2", [128, 16], F32)
        o_bre, o_bim = offA["bre"][0], offA["bim"][0]
        for gp in range(16):
            bre = PA[:, o_bre + gp * 16:o_bre + gp * 16 + 16]
            bim = PA[:, o_bim + gp * 16:o_bim + gp * 16 + 16]
            fr, fi = s5t[:, FR, gp:gp + 1], s5t[:, FI, gp:gp + 1]
            for g2 in range(2):
                rs = slice(64 * g2, 64 * g2 + 64)
                cs = slice(32 * (gp % 3) + 16 * g2, 32 * (gp % 3) + 16 * g2 + 16)
                op("dve", lambda e, rs=rs, bim=bim, fi=fi: e.tensor_scalar(out=bb1[rs, :], in0=bim[rs, :], scalar1=fi[rs, :], scalar2=None, op0=ALU.mult), reads=[PA, s5t], writes=[bb1])
                op("dve", lambda e, rs=rs, cs=cs, bre=bre, fr=fr: e.scalar_tensor_tensor(out=BD[rs, 0, cs], in0=bre[rs, :], scalar=fr[rs, :], in1=bb1[rs, :], op0=ALU.mult, op1=ALU.subtract), reads=[PA, s5t, bb1], writes=[BD])
                op("dve", lambda e, rs=rs, bre=bre, fi=fi: e.tensor_scalar(out=bb2[rs, :], in0=bre[rs, :], scalar1=fi[rs, :], scalar2=-1.0, op0=ALU.mult, op1=ALU.mult), reads=[PA, s5t], writes=[bb2])
                op("dve", lambda e, rs=rs, cs=cs, bim=bim, fr=fr: e.scalar_tensor_tensor(out=bb1[rs, :], in0=bim[rs, :], scalar=fr[rs, :], in1=bb2[rs, :], op0=ALU.mult, op1=ALU.subtract), reads=[PA, s5t, bb2], writes=[bb1])
                op("dve", lambda e, rs=rs, cs=cs: e.tensor_scalar(out=BD[rs, 1, cs], in0=bb1[rs, :], scalar1=-1.0, scalar2=None, op0=ALU.mult), reads=[bb1], writes=[BD])
            if gp % 3 == 2 or gp == 15:
                tl = gp // 3
                m = 96 if tl < 5 else 32
                pb = bank()
                for w_ in range(2):
                    op("pe", lambda e, pb=pb, w_=w_, m=m: e.transpose(pb[0:m, w_ * 128:(w_ + 1) * 128], BD[:, w_, 0:m], P("ident")), reads=[BD, PA], writes=[pb])
                op("act", lambda e, pb=pb, tl=tl, m=m: e.activation(out=BT[0:m, tl, :, :], in_=pb[0:m, 0:256].rearrange("p (w c) -> p w c", w=2), func=AF.Copy), reads=[pb], writes=[BT])

        tap("BT", BT, BT[:])
        xtok = [sb(f"xtok{i}", [128, D], F32) for i in range(2)]
        tokA = sb("tokA", [128, D], F32)
        tokB = sb("tokB", [128, D], F32)
        sst = sb("sst", [128, 8], F32)
        xnT = sb("xnT", [128, 8, NCOL], BF16)
        U = sb("U", [128, 6, NB, TC], F32)
        Ub = sb("Ub", [128, 6, NB, TC], BF16)
        Sb = sb("Sb", [128, 2, 16, NB, SUB], BF16)
        CTb = sb("CTb", [128, 2, 512], BF16)
        op("act", lambda e: e.activation(out=CTb[:, 0, :], in_=P("ctre"), func=AF.Copy), reads=[PA], writes=[CTb])
        op("act", lambda e: e.activation(out=CTb[:, 1, :], in_=P("ctim"), func=AF.Copy), reads=[PA], writes=[CTb])
        QB = [sb(f"qb{i}", [128, NB, TC + 1], F32) for i in range(14)]
        S = sb("S", [128, 2, 16, NB, SUB + 1], F32)
        stmp = sb("stmp", [128, 2, 2, 16, NB], F32)
        Y = sb("Y", [128, 6, NB, SUB], F32)
        Y1 = sb("Y1", [128, 6, NB, SUB], F32)
        Y2 = Y1
        Z = sb("Z", [128, 6, NB, SUB], F32)
        Zb = sb("Zb", [128, 6, NB, SUB], BF16)
        mixin = sb("mixin", [128, 10, NB, TC], BF16)
        mixT = sb("mixT", [128, 8, NCOL], F32)
        ST = [sb(f"ST{j}", [128, NB, 64], F32) for j in range(4)]
        XWA = sb("XWA", [128, NCOL], F32)
        TH = sb("TH", [128, NCOL], F32)
        SGX = sb("SGX", [128, NCOL], F32)
        names = ["qr", "qk", "qv", "sgw", "aa", "gg", "kkt", "t1", "t2", "kap", "kp", "bet", "bon",
                 "cum", "Pm", "Pinv", "oT", "cen"]
        R_ = {n: sb("r_" + n, [128, NCOL], F32) for n in names}
        R_["dd"] = R_["t2"]
        KR = sb("KR", [128, 2, NCOL], BF16)
        OUTS = ["Pm", "bon", "gg"]
        KRs = [KR, sb("KR1", [128, 2, NCOL], BF16)]
        identb = sb("identb", [128, 128], BF16)
        op("act", lambda e: e.activation(out=identb[:], in_=P("ident"), func=AF.Copy), reads=[PA], writes=[identb])
        STb = [sb(f"STb{j}", [128, NB, 64], BF16) for j in range(4)]
        RS13, RS48 = [], []
        t1b = sb("r_t1b", [128, NCOL], F32)
        t2b = sb("r_t2b", [128, NCOL], F32)
        for par in range(2):
            d13 = dict(R_)
            if par == 1:
                for n_ in OUTS:
                    d13[n_] = sb("r1_" + n_, [128, NCOL], F32)
            d13["lw"] = d13["sgw"]
            d13["Ppv"] = d13["t2"]
            d13["bt"] = sb(f"r{par}_btb", [128, NCOL], BF16)
            d13["kt"] = sb(f"r{par}_ktb", [128, NCOL], BF16)
            d13["qvb"] = sb(f"r{par}_qvb", [128, NCOL], BF16)
            RS13.append(d13)
            d48 = {n_: d13[n_] for n_ in OUTS}
            d48["bt"] = d13["bt"]
            d48["kt"] = d13["kt"]
            d48["qvb"] = d13["qvb"]
            d48["oT"] = R_["oT"]
            d48["cen"] = R_["cen"]
            d48["t1"] = t1b
            d48["t2"] = t2b
            RS48.append(d48)
        Vt = sb("Vt", [128, NB, 64], BF16)
        Btk = sb("Btk", [128, NB, 64], BF16)
        Ktk = sb("Ktk", [128, NB, 64], BF16)
        GbS = sb("GbS", [128, NB, 128], BF16)
        GkS = sb("GkS", [128, NB, 128], BF16)
        Xa = [sb(f"Xa{i}", [128, NB, 64], BF16) for i in range(2)]
        XTa = [sb(f"XTa{i}", [128, NB, 64], BF16) for i in range(2)]
        Wt = sb("Wt", [128, NB, 64], F32)
        Wtb = sb("Wtb", [128, NB, 64], BF16)
        Rn = sb("Rn", [128, NB, 64], BF16)
        Uu = sb("Uu", [128, NB, 64], BF16)
        omka = sb("omka", [128, 4], F32)

        op("dve", lambda e: e.tensor_scalar(out=omka[:], in0=P("ka"), scalar1=-1.0, scalar2=1.0, op0=ALU.mult, op1=ALU.add), reads=[PA], writes=[omka])

        def rstd_from_ss(col):
            op("dve", lambda e: e.tensor_scalar(out=sst[:, col:col + 1], in0=sst[:, col:col + 1], scalar1=1.0 / D, scalar2=1e-6, op0=ALU.mult, op1=ALU.add), reads=[sst], writes=[sst])
            op("act", lambda e: e.activation(out=sst[:, col:col + 1], in_=sst[:, col:col + 1], func=AF.Sqrt), reads=[sst], writes=[sst])
            op("dve", lambda e: e.reciprocal(out=sst[:, col:col + 1], in_=sst[:, col:col + 1]), reads=[sst], writes=[sst])

        ident = P("ident")
        onesblk = P("onesblk")

        for it in range(nit):
            t0 = it * TC
            for tt in range(2):
                for b2 in range(2):
                    dma("sp", xtok[tt], xtok[tt][64 * b2:64 * b2 + 64, :], x[2 * tt + b2, t0:t0 + TC, :])
            for tt in range(2):
                op("dve", lambda e, tt=tt: e.scalar_tensor_tensor(out=tokA[:], in0=xtok[tt][:], scalar=1.0, in1=xtok[tt][:], op0=ALU.mult, op1=ALU.mult, accum_out=sst[:, tt:tt + 1]), reads=[xtok[tt]], writes=[tokA, sst])
                rstd_from_ss(tt)
                op("dve", lambda e, tt=tt: e.tensor_scalar(out=tokA[:], in0=xtok[tt][:], scalar1=sst[:, tt:tt + 1], scalar2=None, op0=ALU.mult), reads=[xtok[tt], sst], writes=[tokA])
                for kh in range(2):
                    pb = bank()
                    for k4 in range(4):
                        k = kh * 4 + k4
                        op("pe", lambda e, pb=pb, k=k, k4=k4: e.transpose(pb[:, k4 * 128:(k4 + 1) * 128], tokA[:, k * 128:(k + 1) * 128], ident), reads=[tokA, PA], writes=[pb])
                    gpre = P("gpre", kh * 4, kh * 4 + 4).unsqueeze(2).to_broadcast([128, 4, 128])
                    op("dve", lambda e, pb=pb, kh=kh, tt=tt, gpre=gpre: e.tensor_tensor(out=xnT[:, kh * 4:kh * 4 + 4, tt * 128:(tt + 1) * 128], in0=pb[:, :].rearrange("p (k c) -> p k c", k=4), in1=gpre, op=ALU.mult), reads=[pb, PA], writes=[xnT])
            for ct in range(20):
                pb = bank()
                if ct < 6:
                    c0, m = 96 * ct, TR[ct]
                else:
                    c0, m = 512 + 128 * (ct - 6), 128
                for k in range(8):
                    op("pe", lambda e, pb=pb, k=k, c0=c0, m=m: e.matmul(pb[0:m, 0:NCOL], lhsT=Win[:, k, c0:c0 + m], rhs=xnT[:, k, :], start=(k == 0), stop=(k == 7)), reads=[Win, xnT], writes=[pb])
                if ct < 6:
                    op("act", lambda e, pb=pb, ct=ct, m=m: e.activation(out=U[0:m, ct, :, :], in_=pb[0:m, 0:NCOL].rearrange("p (b t) -> p b t", b=NB), func=AF.Copy), reads=[pb], writes=[U])
                    op("act", lambda e, pb=pb, ct=ct, m=m: e.activation(out=Ub[0:m, ct, :, :], in_=pb[0:m, 0:NCOL].rearrange("p (b t) -> p b t", b=NB), func=AF.Copy), reads=[pb], writes=[Ub])
                else:
                    qb = QB[ct - 6]
                    op("act", lambda e, pb=pb, qb=qb: e.activation(out=qb[:, :, 1:TC + 1], in_=pb[:, 0:NCOL].rearrange("p (b t) -> p b t", b=NB), func=AF.Copy), reads=[pb], writes=[qb])

            def s5_front(sc):
                tsl = slice(sc * SUB, (sc + 1) * SUB)
                for gp in range(16):
                    tl, q = gp // 3, gp % 3
                    pb = bank()
                    for w_ in range(2):
                        op("pe", lambda e, pb=pb, tl=tl, q=q, w_=w_: e.matmul(pb[:, w_ * NB * SUB:(w_ + 1) * NB * SUB], lhsT=BT[32 * q:32 * q + 32, tl, w_, :], rhs=Ub[32 * q:32 * q + 32, tl, :, tsl], start=True, stop=True), reads=[BT, Ub], writes=[pb])
                    op("act", lambda e, pb=pb, gp=gp: e.activation(out=S[:, :, gp, :, 1:SUB + 1], in_=pb[:, 0:2 * NB * SUB].rearrange("p (c b t) -> p c b t", c=2, b=NB), func=AF.Copy), reads=[pb], writes=[S])
                for t in range(SUB):
                    prev = S[:, :, :, :, t]
                    cur = S[:, :, :, :, t + 1]
                    a1 = Acf[:, 0, :, :].unsqueeze(3).to_broadcast([128, 2, 16, NB])
                    op(SCAN_ENG, lambda e, prev=prev, a1=a1: e.tensor_tensor(out=stmp[:, 0, :, :, :], in0=prev, in1=a1, op=ALU.mult), reads=[S, Acf], writes=[stmp])
                    for c in range(2):
                        a2 = Acf[:, 1, c, :].unsqueeze(2).to_broadcast([128, 16, NB])
                        op(SCAN_ENG, lambda e, c=c, a2=a2, t=t: e.tensor_tensor(out=stmp[:, 1, c, :, :], in0=S[:, 1 - c, :, :, t], in1=a2, op=ALU.mult), reads=[S, Acf], writes=[stmp])
                    op(SCAN_ENG, lambda e: e.tensor_tensor(out=stmp[:, 0, :, :, :], in0=stmp[:, 0, :, :, :], in1=stmp[:, 1, :, :, :], op=ALU.add), reads=[stmp], writes=[stmp])
                    op(SCAN_ENG, lambda e, cur=cur: e.tensor_tensor(out=cur, in0=cur, in1=stmp[:, 0, :, :, :], op=ALU.add), reads=[stmp, S], writes=[S])
                tap("S", S, S[:])
                tap("U", U, U[:])

            def s5_back(sc):
                tsl = slice(sc * SUB, (sc + 1) * SUB)
                op("act", lambda e: e.activation(out=Sb[:, :, :, :, :], in_=S[:, :, :, :, 1:SUB + 1], func=AF.Copy), reads=[S], writes=[Sb])
                for tl in range(6):
                    pb = bank()
                    for q in range(3 if tl < 5 else 1):
                        gp = tl * 3 + q
                        oc, nct = offA["ctre"][0], offA["ctim"][0]
                        op("pe", lambda e, pb=pb, q=q, gp=gp, oc=oc: e.matmul(pb[32 * q:32 * q + 32, 0:NB * SUB], lhsT=CTb[:, 0, gp * 32:gp * 32 + 32], rhs=Sb[:, 0, gp, :, :], start=True, stop=False), reads=[CTb, Sb], writes=[pb])
                        op("pe", lambda e, pb=pb, q=q, gp=gp, nct=nct: e.matmul(pb[32 * q:32 * q + 32, 0:NB * SUB], lhsT=CTb[:, 1, gp * 32:gp * 32 + 32], rhs=Sb[:, 1, gp, :, :], start=False, stop=True), reads=[CTb, Sb], writes=[pb])
                    op("dve", lambda e, pb=pb, tl=tl: e.scalar_tensor_tensor(out=Y[:, tl, :, :], in0=U[:, tl, :, tsl], scalar=P("dskip", tl, tl + 1), in1=pb[:, 0:NB * SUB].rearrange("p (b t) -> p b t", b=NB), op0=ALU.mult, op1=ALU.add), reads=[U, PA, pb], writes=[Y])
                tap("Y", Y, Y[:])
                op(SCAN_ENG, lambda e: e.tensor_copy(out=S[:, :, :, :, 0], in_=S[:, :, :, :, SUB]), reads=[S], writes=[S])
                fl = lambda b_: b_[:].rearrange("p a b t -> p (a b t)")
                op("dve", lambda e: e.tensor_tensor(out=fl(Y1), in0=fl(Y), in1=fl(Y), op=ALU.mult), reads=[Y], writes=[Y1])
                op("dve", lambda e: e.tensor_scalar(out=fl(Y1), in0=fl(Y1), scalar1=0.044715, scalar2=1.0, op0=ALU.mult, op1=ALU.add), reads=[Y1], writes=[Y1])
                op("dve", lambda e: e.tensor_tensor(out=fl(Y1), in0=fl(Y1), in1=fl(Y), op=ALU.mult), reads=[Y, Y1], writes=[Y1])
                op("act", lambda e: e.activation(out=fl(Y2), in_=fl(Y1), func=AF.Sigmoid, scale=1.5957691216057308), reads=[Y1], writes=[Y2])
                op("dve", lambda e: e.tensor_tensor(out=fl(Z), in0=fl(Y), in1=fl(Y2), op=ALU.mult), reads=[Y, Y2], writes=[Z])
                op("act", lambda e: e.activation(out=fl(Zb), in_=fl(Z), func=AF.Copy), reads=[Z], writes=[Zb])
                for ct in range(6):
                    pb = bank()
                    m = TR[ct]
                    for k in range(6):
                        op("pe", lambda e, pb=pb, k=k, ct=ct, m=m: e.matmul(pb[0:m, 0:NB * SUB], lhsT=Wglu[0:TR[k], k, ct * 96:ct * 96 + m], rhs=Zb[0:TR[k], k, :, :], start=(k == 0), stop=(k == 5)), reads=[Wglu, Zb], writes=[pb])
                    op("act", lambda e, pb=pb, ct=ct, m=m: e.activation(out=Y2[0:m, ct, :, :], in_=pb[0:m, 0:NB * SUB].rearrange("p (b t) -> p b t", b=NB), func=AF.Sigmoid, bias=P("bglu", ct, ct + 1, rows=slice(0, m))), reads=[pb, PA], writes=[Y2])
                    op("dve", lambda e, ct=ct, m=m: e.tensor_tensor(out=mixin[0:m, ct, :, tsl], in0=Z[0:m, ct, :, :], in1=Y2[0:m, ct, :, :], op=ALU.mult), reads=[Z, Y2], writes=[mixin])

            def shift(dst, qb, mucol):
                v3 = lambda b_: b_[:].rearrange("p (b t) -> p b t", b=NB)
                op("dve", lambda e: e.tensor_tensor(out=v3(R_["dd"]), in0=qb[:, :, 0:TC], in1=qb[:, :, 1:TC + 1], op=ALU.subtract), reads=[qb], writes=[R_["dd"]])
                op("dve", lambda e: e.scalar_tensor_tensor(out=v3(dst), in0=v3(R_["dd"]), scalar=P("mu", mucol, mucol + 1), in1=qb[:, :, 1:TC + 1], op0=ALU.mult, op1=ALU.add), reads=[R_["dd"], qb, PA], writes=[dst])
                op("dve", lambda e: e.tensor_copy(out=qb[:, :, 0:1], in_=qb[:, :, TC:TC + 1]), reads=[qb], writes=[qb])

            shift(XWA, QB[12], 12)
            op("act", lambda e: e.activation(out=TH[0:64, :], in_=XWA[0:64, :], func=AF.Tanh), reads=[XWA], writes=[TH])
            shift(SGX, QB[13], 13)
            op("act", lambda e: e.activation(out=SGX[:], in_=SGX[:], func=AF.Sigmoid), reads=[SGX], writes=[SGX])
            tt2 = lambda o, a, b, f, eng="dve": op(eng, lambda e: e.tensor_tensor(out=o[:], in0=a[:], in1=b[:], op=f), reads=[a, b], writes=[o])
            def rwkv_s13(j):
                r = RS13[j % 2]
                KR = KRs[j % 2]
                shift(r["qr"], QB[j], j)
                shift(r["qk"], QB[4 + j], 4 + j)
                shift(r["qv"], QB[8 + j], 8 + j)
                op("act", lambda e: e.activation(out=r["qvb"][:], in_=r["qv"][:], func=AF.Copy), reads=[r["qv"]], writes=[r["qvb"]])
                yield
                cs = slice(j * 128, (j + 1) * 128)
                pbw = bank()
                pba = bank()
                op("pe", lambda e, pbw=pbw, cs=cs: e.matmul(pbw[:, 0:NCOL], lhsT=P("wau", rows=slice(0, 64))[:, cs], rhs=TH[0:64, :], start=True, stop=True), reads=[PA, TH], writes=[pbw])
                op("pe", lambda e, pba=pba, cs=cs: e.matmul(pba[:, 0:NCOL], lhsT=P("wau", rows=slice(64, 128))[:, cs], rhs=XWA[64:128, :], start=True, stop=True), reads=[PA, XWA], writes=[pba])
                op("act", lambda e, pbw=pbw, j=j: e.activation(out=r["sgw"][:], in_=pbw[:, 0:NCOL], func=AF.Sigmoid, bias=P("w0", j, j + 1)), reads=[pbw, PA], writes=[r["sgw"]])
                op("act", lambda e, pba=pba, j=j: e.activation(out=r["aa"][:], in_=pba[:, 0:NCOL], func=AF.Sigmoid, bias=P("a0", j, j + 1)), reads=[pba, PA], writes=[r["aa"]])
                pb = bank()
                op("pe", lambda e, pb=pb, cs=cs: e.matmul(pb[:, 0:NCOL], lhsT=P("gup")[:, cs], rhs=SGX[:], start=True, stop=True), reads=[PA, SGX], writes=[pb])
                op("act", lambda e, pb=pb: e.activation(out=r["gg"][:], in_=pb[:, 0:NCOL], func=AF.Copy), reads=[pb], writes=[r["gg"]])
                yield
                if KSTOP <= 1:
                    return
                op("dve", lambda e: e.tensor_scalar(out=r["lw"][:], in0=r["sgw"][:], scalar1=-0.6065306597126334, scalar2=None, op0=ALU.mult), reads=[r["sgw"]], writes=[r["lw"]])
                op("dve", lambda e, j=j: e.tensor_scalar(out=r["kkt"][:], in0=r["qk"][:], scalar1=P("kk", j, j + 1), scalar2=None, op0=ALU.mult), reads=[r["qk"], PA], writes=[r["kkt"]])
                tt2(r["t1"], r["kkt"], r["kkt"], ALU.mult)
                pb = bank()
                op("pe", lambda e, pb=pb: e.matmul(pb[:, 0:NCOL], lhsT=onesblk, rhs=r["t1"][:], start=True, stop=True), reads=[PA, r["t1"]], writes=[pb])
                op("dve", lambda e, pb=pb: e.tensor_scalar(out=r["t2"][:], in0=pb[:, 0:NCOL], scalar1=1e-24, scalar2=None, op0=ALU.max), reads=[pb], writes=[r["t2"]])
                op("act", lambda e: e.activation(out=r["t2"][:], in_=r["t2"][:], func=AF.Sqrt), reads=[r["t2"]], writes=[r["t2"]])
                op("dve", lambda e: e.reciprocal(out=r["t2"][:], in_=r["t2"][:]), reads=[r["t2"]], writes=[r["t2"]])
                tt2(r["kap"], r["kkt"], r["t2"], ALU.mult)
                yield
                op("dve", lambda e, j=j: e.tensor_scalar(out=r["t1"][:], in0=r["aa"][:], scalar1=P("ka", j, j + 1), scalar2=omka[:, j:j + 1], op0=ALU.mult, op1=ALU.add), reads=[r["aa"], PA, omka], writes=[r["t1"]])
                tt2(r["kp"], r["qk"], r["t1"], ALU.mult)
                tt2(r["bet"], r["kap"], r["aa"], ALU.mult)
                op("dve", lambda e, j=j: e.scalar_tensor_tensor(out=r["t1"][:], in0=r["qr"][:], scalar=P("rk", j, j + 1), in1=r["kp"][:], op0=ALU.mult, op1=ALU.mult), reads=[r["qr"], r["kp"], PA], writes=[r["t1"]])
                pb = bank()
                op("pe", lambda e, pb=pb: e.matmul(pb[:, 0:NCOL], lhsT=onesblk, rhs=r["t1"][:], start=True, stop=True), reads=[PA, r["t1"]], writes=[pb])
                op("dve", lambda e, pb=pb: e.tensor_tensor(out=r["bon"][:], in0=pb[:, 0:NCOL], in1=r["qv"][:], op=ALU.mult), reads=[pb, r["qv"]], writes=[r["bon"]])
                yield
                if KSTOP <= 2:
                    return
                op("dve", lambda e: e.tensor_tensor_scan(out=r["cum"][:], data0=P("scanmask"), data1=r["lw"][:], initial=0.0, op0=ALU.mult, op1=ALU.add), reads=[PA, r["lw"]], writes=[r["cum"]])
                op("act", lambda e: e.activation(out=r["Pm"][:], in_=r["cum"][:], func=AF.Exp), reads=[r["cum"]], writes=[r["Pm"]])
                op("act", lambda e: e.activation(out=r["Pinv"][:], in_=r["cum"][:], func=AF.Exp, scale=-1.0), reads=[r["cum"]], writes=[r["Pinv"]])
                tt2(r["t2"], r["cum"], r["lw"], ALU.subtract)
                op("act", lambda e: e.activation(out=r["Ppv"][:], in_=r["t2"][:], func=AF.Exp), reads=[r["t2"]], writes=[r["Ppv"]])
                op("dve", lambda e: e.tensor_tensor(out=KR[:, 0, :], in0=r["kap"][:], in1=r["Ppv"][:], op=ALU.mult), reads=[r["kap"], r["Ppv"]], writes=[KR])
                op("dve", lambda e: e.tensor_tensor(out=KR[:, 1, :], in0=r["qr"][:], in1=r["Pm"][:], op=ALU.mult), reads=[r["qr"], r["Pm"], KR], writes=[KR])
                tt2(r["bt"], r["bet"], r["Pinv"], ALU.mult)
                tt2(r["kt"], r["kp"], r["Pinv"], ALU.mult)
                yield
                if KSTOP <= 3:
                    return
                yield

            def rwkv_s48(j):
                r = RS48[j % 2]
                KR = KRs[j % 2]
                HS = [slice(0, 64), slice(64, 128)]
                f2 = lambda b_, rs: b_[rs, :, :].rearrange("p b c -> p (b c)")
                idb = [identb[HS[h2], 64 * h2:64 * h2 + 64] for h2 in range(2)]
                BCS = [slice(b * 64, (b + 1) * 64) for b in range(NB)]
                pv = [bank(), bank()]
                pk2 = [bank(), bank()]
                for b in range(NB):
                    bc = BCS[b]
                    for h2 in range(2):
                        rs = HS[h2]
                        op("pe", lambda e: e.matmul(pv[h2][rs, b * 64:(b + 1) * 64], lhsT=r["qvb"][rs, bc], rhs=idb[h2], start=True, stop=True), reads=[r["qvb"], identb], writes=[pv[h2]])
                        op("pe", lambda e: e.matmul(pv[h2][rs, 256 + b * 64:256 + (b + 1) * 64], lhsT=r["bt"][rs, bc], rhs=idb[h2], start=True, stop=True), reads=[r["bt"], identb], writes=[pv[h2]])
                        op("pe", lambda e: e.matmul(pk2[h2][rs, b * 64:(b + 1) * 64], lhsT=r["kt"][rs, bc], rhs=idb[h2], start=True, stop=True), reads=[r["kt"], identb], writes=[pk2[h2]])
                for h2 in range(2):
                    rs = HS[h2]
                    op("act", lambda e: e.activation(out=f2(Vt, rs), in_=pv[h2][rs, 0:256], func=AF.Copy), reads=[pv[h2]], writes=[Vt])
                    op("act", lambda e: e.activation(out=f2(Btk, rs), in_=pv[h2][rs, 256:512], func=AF.Copy), reads=[pv[h2]], writes=[Btk])
                    op("act", lambda e: e.activation(out=f2(Ktk, rs), in_=pk2[h2][rs, 0:256], func=AF.Copy), reads=[pk2[h2]], writes=[Ktk])
                yield
                if KSTOP <= 4:
                    return
                pgb = [bank(), bank()]
                pgk = [bank(), bank()]
                pl = [bank(), bank()]
                for b in range(NB):
                    bc = BCS[b]
                    for h2 in range(2):
                        rs = HS[h2]
                        op("pe", lambda e: e.matmul(pl[h2][rs, b * 64:(b + 1) * 64], lhsT=KR[rs, 0, bc], rhs=r["bt"][rs, bc], start=True, stop=True), reads=[r["bt"], KR], writes=[pl[h2]])
                        op("pe", lambda e: e.matmul(pgb[h2][rs, b * 128:(b + 1) * 128], lhsT=r["bt"][rs, bc], rhs=KR[rs, :, bc], start=True, stop=True), reads=[r["bt"], KR], writes=[pgb[h2]])
                        op("pe", lambda e: e.matmul(pgk[h2][rs, b * 128:(b + 1) * 128], lhsT=r["kt"][rs, bc], rhs=KR[rs, :, bc], start=True, stop=True), reads=[r["kt"], KR], writes=[pgk[h2]])
                for h2 in range(2):
                    rs = HS[h2]
                    mG = P("maskG", rows=rs).unsqueeze(1).to_broadcast([64, NB, 128])
                    mL = P("maskL", rows=rs).unsqueeze(1).to_broadcast([64, NB, 64])
                    i64 = P("ident", 64 * h2, 64 * h2 + 64, rows=rs).unsqueeze(1).to_broadcast([64, NB, 64])
                    op("dve", lambda e: e.tensor_tensor(out=Xa[0][rs, :, :], in0=pl[h2][rs, 0:256].rearrange("p (u c) -> p u c", u=NB), in1=mL, op=ALU.mult), reads=[pl[h2], PA], writes=[Xa[0]])
                    op("dve", lambda e: e.tensor_tensor(out=GbS[rs, :, :], in0=pgb[h2][rs, :].rearrange("p (u c) -> p u c", u=NB), in1=mG, op=ALU.mult), reads=[pgb[h2], PA], writes=[GbS])
                    op("dve", lambda e: e.tensor_tensor(out=Wt[rs, :, :], in0=i64, in1=GbS[rs, :, 0:64], op=ALU.subtract), reads=[PA, GbS], writes=[Wt])
                    op("act", lambda e: e.activation(out=XTa[0][rs, :, :], in_=GbS[rs, :, 0:64], func=AF.Copy), reads=[GbS], writes=[XTa[0]])
                    op("act", lambda e: e.activation(out=Wtb[rs, :, :], in_=Wt[rs, :, :], func=AF.Copy), reads=[Wt], writes=[Wtb])
                    op("dve", lambda e: e.tensor_tensor(out=GkS[rs, :, :], in0=pgk[h2][rs, :].rearrange("p (u c) -> p u c", u=NB), in1=mG, op=ALU.mult), reads=[pgk[h2], PA], writes=[GkS])
                yield
                if KSTOP <= 5:
                    return
                for lvl in range(1, 6):
                    Xp = Xa[(lvl - 1) % 2]
                    Xn = Xa[lvl % 2]
                    XTn = XTa[lvl % 2]
                    XTp_buf, XTp_ap = XTa[(lvl - 1) % 2], (lambda rs, b, t_=XTa[(lvl - 1) % 2]: t_[rs, b, :])
                    px = [bank(), bank()]
                    for b in range(NB):
                        for h2 in range(2):
                            rs = HS[h2]
                            op("pe", lambda e: e.matmul(px[h2][rs, b * 64:(b + 1) * 64], lhsT=XTp_ap(rs, b), rhs=Xp[rs, b, :], start=True, stop=True), reads=[Xp, XTp_buf], writes=[px[h2]])
                    for h2 in range(2):
                        rs = HS[h2]
                        op("act", lambda e: e.activation(out=f2(Xn, rs), in_=px[h2][rs, 0:256], func=AF.Copy), reads=[px[h2]], writes=[Xn])
                    if lvl < 5:
                        pxt = [bank(), bank()]
                        for b in range(NB):
                            for h2 in range(2):
                                rs = HS[h2]
                                op("pe", lambda e: e.matmul(pxt[h2][rs, b * 64:(b + 1) * 64], lhsT=Xp[rs, b, :], rhs=XTp_ap(rs, b), start=True, stop=True), reads=[Xp, XTp_buf], writes=[pxt[h2]])
                        for h2 in range(2):
                            rs = HS[h2]
                            op("act", lambda e: e.activation(out=f2(XTn, rs), in_=pxt[h2][rs, 0:256], func=AF.Copy), reads=[pxt[h2]], writes=[XTn])
                    pw = [bank(), bank()]
                    for b in range(NB):
                        for h2 in range(2):
                            rs = HS[h2]
                            op("pe", lambda e: e.matmul(pw[h2][rs, b * 64:(b + 1) * 64], lhsT=Xn[rs, b, :], rhs=Wtb[rs, b, :], start=True, stop=True), reads=[Xn, Wtb], writes=[pw[h2]])
                    for h2 in range(2):
                        rs = HS[h2]
                        op("dve", lambda e: e.tensor_tensor(out=f2(Wt, rs), in0=pw[h2][rs, 0:256], in1=f2(Wt, rs), op=ALU.add), reads=[pw[h2], Wt], writes=[Wt])
                        op("act", lambda e: e.activation(out=f2(Wtb, rs), in_=f2(Wt, rs), func=AF.Copy), reads=[Wt], writes=[Wtb])
                    yield
                yield
                if KSTOP <= 6:
                    return
                STj = ST[j]
                pr = [bank(), bank()]
                for b in range(NB):
                    bc = BCS[b]
                    for h2 in range(2):
                        rs = HS[h2]
                        op("pe", lambda e: e.matmul(pr[h2][rs, b * 64:(b + 1) * 64], lhsT=KR[rs, 0, bc], rhs=STb[j][rs, b, :], start=True, stop=False), reads=[KR, STb[j]], writes=[pr[h2]])
                        op("pe", lambda e: e.matmul(pr[h2][rs, b * 64:(b + 1) * 64], lhsT=GkS[rs, b, 0:64], rhs=Vt[rs, b, :], start=False, stop=True), reads=[GkS, Vt], writes=[pr[h2]])
                for h2 in range(2):
                    rs = HS[h2]
                    op("act", lambda e: e.activation(out=f2(Rn, rs), in_=pr[h2][rs, 0:256], func=AF.Copy, scale=-1.0), reads=[pr[h2]], writes=[Rn])
                pu = [bank(), bank()]
                for b in range(NB):
                    for h2 in range(2):
                        rs = HS[h2]
                        op("pe", lambda e: e.matmul(pu[h2][rs, b * 64:(b + 1) * 64], lhsT=Wtb[rs, b, :], rhs=Rn[rs, b, :], start=True, stop=True), reads=[Wtb, Rn], writes=[pu[h2]])
                for h2 in range(2):
                    rs = HS[h2]
                    op("act", lambda e: e.activation(out=f2(Uu, rs), in_=pu[h2][rs, 0:256], func=AF.Copy), reads=[pu[h2]], writes=[Uu])
                yield
                if KSTOP <= 7:
                    return
                po = [bank(), bank()]
                psn = [bank(), bank()]
                for b in range(NB):
                    bc = BCS[b]
                    for h2 in range(2):
                        rs = HS[h2]
                        op("pe", lambda e: e.matmul(po[h2][rs, bc], lhsT=STb[j][rs, b, :], rhs=KR[rs, 1, bc], start=True, stop=False), reads=[STb[j], KR], writes=[po[h2]])
                        op("pe", lambda e: e.matmul(po[h2][rs, bc], lhsT=Uu[rs, b, :], rhs=GbS[rs, b, 64:128], start=False, stop=False), reads=[Uu, GbS], writes=[po[h2]])
                        op("pe", lambda e: e.matmul(po[h2][rs, bc], lhsT=Vt[rs, b, :], rhs=GkS[rs, b, 64:128], start=False, stop=True), reads=[Vt, GkS], writes=[po[h2]])
                        op("pe", lambda e: e.matmul(psn[h2][rs, bc], lhsT=Btk[rs, b, :], rhs=Uu[rs, b, :], start=True, stop=False), reads=[Btk, Uu], writes=[psn[h2]])
                        op("pe", lambda e: e.matmul(psn[h2][rs, bc], lhsT=Ktk[rs, b, :], rhs=Vt[rs, b, :], start=False, stop=True), reads=[Ktk, Vt], writes=[psn[h2]])
                for h2 in range(2):
                    rs = HS[h2]
                    op("act", lambda e: e.activation(out=r["oT"][rs, :], in_=po[h2][rs, 0:NCOL], func=AF.Copy), reads=[po[h2]], writes=[r["oT"]])
                    op("dve", lambda e: e.tensor_tensor(out=r["t1"][rs, :], in0=psn[h2][rs, 0:NCOL], in1=f2(STj, rs), op=ALU.add), reads=[psn[h2], STj], writes=[r["t1"]])
                    pT = r["Pm"][rs, :].rearrange("p (b t) -> p b t", b=NB)[:, :, TC - 1:TC].to_broadcast([64, NB, 64])
                    op("dve", lambda e: e.tensor_tensor(out=STj[rs, :, :], in0=r["t1"][rs, :].rearrange("p (b c) -> p b c", b=NB), in1=pT, op=ALU.mult), reads=[r["t1"], r["Pm"]], writes=[STj])
                    op("act", lambda e: e.activation(out=STb[j][rs, :, :], in_=STj[rs, :, :], func=AF.Copy), reads=[STj], writes=[STb[j]])
                yield
                if KSTOP <= 8:
                    return
                pb = bank()
                op("pe", lambda e, pb=pb: e.matmul(pb[:, 0:NCOL], lhsT=onesblk, rhs=r["oT"][:], start=True, stop=True), reads=[PA, r["oT"]], writes=[pb])
                op("dve", lambda e, pb=pb: e.scalar_tensor_tensor(out=r["cen"][:], in0=pb[:, 0:NCOL], scalar=-1.0 / 64, in1=r["oT"][:], op0=ALU.mult, op1=ALU.add), reads=[pb, r["oT"]], writes=[r["cen"]])
                tt2(r["t1"], r["cen"], r["cen"], ALU.mult)
                pb = bank()
                op("pe", lambda e, pb=pb: e.matmul(pb[:, 0:NCOL], lhsT=onesblk, rhs=r["t1"][:], start=True, stop=True), reads=[PA, r["t1"]], writes=[pb])
                op("dve", lambda e, pb=pb: e.tensor_scalar(out=r["t2"][:], in0=pb[:, 0:NCOL], scalar1=1.0 / 64, scalar2=GN_EPS, op0=ALU.mult, op1=ALU.add), reads=[pb], writes=[r["t2"]])
                op("act", lambda e: e.activation(out=r["t2"][:], in_=r["t2"][:], func=AF.Sqrt), reads=[r["t2"]], writes=[r["t2"]])
                op("dve", lambda e: e.reciprocal(out=r["t2"][:], in_=r["t2"][:]), reads=[r["t2"]], writes=[r["t2"]])
                tt2(r["cen"], r["cen"], r["t2"], ALU.mult)
                op("dve", lambda e, j=j: e.tensor_scalar(out=r["cen"][:], in0=r["cen"][:], scalar1=P("gnw", j, j + 1), scalar2=P("gnb", j, j + 1), op0=ALU.mult, op1=ALU.add), reads=[r["cen"], PA], writes=[r["cen"]])
                tt2(r["cen"], r["cen"], r["bon"], ALU.add)
                op("dve", lambda e, j=j: e.tensor_tensor(out=mixin[:, 6 + j, :, :].rearrange("p b t -> p (b t)"), in0=r["cen"][:], in1=r["gg"][:], op=ALU.mult), reads=[r["cen"], r["gg"]], writes=[mixin])

            n_s5 = 0 if "s" in SKIP else TC // SUB
            n_rw = 0 if "r" in SKIP else 4

            def run_all(g):
                for _ in g:
                    pass

            def zipgen(ga, gb):
                alive = [ga, gb]
                while alive:
                    for g in list(alive):
                        try:
                            next(g)
                        except StopIteration:
                            alive.remove(g)

            if n_rw:
                run_all(rwkv_s13(0))
            for i_ in range(4):
                if i_ < n_s5:
                    s5_front(i_)
                if i_ < n_rw:
                    if i_ + 1 < n_rw:
                        zipgen(rwkv_s48(i_), rwkv_s13(i_ + 1))
                    else:
                        run_all(rwkv_s48(i_))
                if i_ < n_s5:
                    s5_back(i_)
            tap("Z", Z, Z[:])
            tap("mixs5", mixin, mixin[:, 0:6, :, :])

            for ct in range(8):
                pb = bank()
                for k in range(10):
                    kr = TR[k] if k < 6 else 128
                    op("pe", lambda e, pb=pb, k=k, ct=ct, kr=kr: e.matmul(pb[:, 0:NCOL], lhsT=Wout[0:kr, k, ct * 128:(ct + 1) * 128], rhs=mixin[0:kr, k, :, :], start=(k == 0), stop=(k == 9)), reads=[Wout, mixin], writes=[pb])
                op("act", lambda e, pb=pb, ct=ct: e.activation(out=mixT[:, ct, :], in_=pb[:, 0:NCOL], func=AF.Copy), reads=[pb], writes=[mixT])
            for tt in range(2):
                for kh in range(2):
                    pb = bank()
                    for k4 in range(4):
                        k = kh * 4 + k4
                        op("pe", lambda e, pb=pb, k=k, k4=k4, tt=tt: e.transpose(pb[:, k4 * 128:(k4 + 1) * 128], mixT[:, k, tt * 128:(tt + 1) * 128], ident), reads=[mixT, PA], writes=[pb])
                    op("dve", lambda e, pb=pb, kh=kh: e.tensor_copy(out=tokB[:, kh * 512:(kh + 1) * 512], in_=pb[:, :]), reads=[pb], writes=[tokB])
                op("dve", lambda e, tt=tt: e.scalar_tensor_tensor(out=tokA[:], in0=tokB[:], scalar=1.0, in1=tokB[:], op0=ALU.mult, op1=ALU.mult, accum_out=sst[:, 2 + tt:3 + tt]), reads=[tokB], writes=[tokA, sst])
                rstd_from_ss(2 + tt)
                op("dve", lambda e, tt=tt: e.scalar_tensor_tensor(out=tokA[:], in0=tokB[:], scalar=sst[:, 2 + tt:3 + tt], in1=P("gBmix"), op0=ALU.mult, op1=ALU.mult), reads=[tokB, sst, PA], writes=[tokA])
                op("dve", lambda e, tt=tt: e.tensor_tensor(out=tokA[:], in0=tokA[:], in1=xtok[tt][:], op=ALU.add), reads=[tokA, xtok[tt]], writes=[tokA])
                for b2 in range(2):
                    dma("sp", None, hsc[2 * tt + b2, t0:t0 + TC, :], tokA[64 * b2:64 * b2 + 64, :], reads=[tokA], out_final=True)
        fw.emit()
    if not do_ffn:
        fw.stack.close()
        return nc

    fw2 = fw
    fw2.final_tokens = []
    with fw2.tscope():
        sb, op, dma = fw2.sb, fw2.op, fw2.dma
        PB = sb("PB", [128, nB], F32)

        def P2(name, c0=0, c1=None):
            o, n = offB[name]
            c1 = n if c1 is None else c1
            return PB[:, o + c0:o + c1]
        Wup = sb("Wup", [128, 8, DFF], BF16)
        Wdn = sb("Wdn", [128, 32, D], BF16)
        banks = [fw2.ps(f"bankb{i}", [128, 512], F32) for i in range(8)]
        bi = [0]

        def bank():
            b = banks[bi[0] % 8]
            bi[0] += 1
            return b
        dma("sp", PB, PB[:], ppb)
        for k in range(8):
            for h in range(2):
                dma("pool", Wup, Wup[:, k, h * 2048:(h + 1) * 2048], w_up[k * 128:(k + 1) * 128, h * 2048:(h + 1) * 2048])
        for k in range(32):
            dma("pool", Wdn, Wdn[:, k, :], w_dn[k * 128:(k + 1) * 128, :])
        htok = [sb(f"htok{i}", [128, D], F32) for i in range(2)]
        tokA = sb("tokA2", [128, D], F32)
        tokB = sb("tokB2", [128, D], F32)
        sst = sb("sst2", [128, 8], F32)
        hnT = sb("hnT", [128, 8, NCOL], BF16)
        hid = sb("hid", [128, 32, NCOL], BF16)
        rl = [sb(f"rl{i}", [128, NCOL], F32) for i in range(2)]
        ffT = sb("ffT", [128, 8, NCOL], F32)
        ident = P2("ident")

        def rstd_from_ss2(col):
            op("dve", lambda e: e.tensor_scalar(out=sst[:, col:col + 1], in0=sst[:, col:col + 1], scalar1=1.0 / D, scalar2=1e-6, op0=ALU.mult, op1=ALU.add), reads=[sst], writes=[sst])
            op("act", lambda e: e.activation(out=sst[:, col:col + 1], in_=sst[:, col:col + 1], func=AF.Sqrt), reads=[sst], writes=[sst])
            op("dve", lambda e: e.reciprocal(out=sst[:, col:col + 1], in_=sst[:, col:col + 1]), reads=[sst], writes=[sst])

        for it in range(nit):
            t0 = it * TC
            for tt in range(2):
                for b2 in range(2):
                    dma("sp", htok[tt], htok[tt][64 * b2:64 * b2 + 64, :], hsc[2 * tt + b2, t0:t0 + TC, :])
            for tt in range(2):
                op("dve", lambda e, tt=tt: e.scalar_tensor_tensor(out=tokA[:], in0=htok[tt][:], scalar=1.0, in1=htok[tt][:], op0=ALU.mult, op1=ALU.mult, accum_out=sst[:, tt:tt + 1]), reads=[htok[tt]], writes=[tokA, sst])
                rstd_from_ss2(tt)
                op("dve", lambda e, tt=tt: e.tensor_scalar(out=tokA[:], in0=htok[tt][:], scalar1=sst[:, tt:tt + 1], scalar2=None, op0=ALU.mult), reads=[htok[tt], sst], writes=[tokA])
                for kh in range(2):
                    pb = bank()
                    for k4 in range(4):
                        k = kh * 4 + k4
                        op("pe", lambda e, pb=pb, k=k, k4=k4: e.transpose(pb[:, k4 * 128:(k4 + 1) * 128], tokA[:, k * 128:(k + 1) * 128], ident), reads=[tokA, PB], writes=[pb])
                    gpre = P2("gpremlp", kh * 4, kh * 4 + 4).unsqueeze(2).to_broadcast([128, 4, 128])
                    op("dve", lambda e, pb=pb, kh=kh, tt=tt, gpre=gpre: e.tensor_tensor(out=hnT[:, kh * 4:kh * 4 + 4, tt * 128:(tt + 1) * 128], in0=pb[:, :].rearrange("p (k c) -> p k c", k=4), in1=gpre, op=ALU.mult), reads=[pb, PB], writes=[hnT])
            for ht in range(32):
                pb = bank()
                for k in range(8):
                    op("pe", lambda e, pb=pb, k=k, ht=ht: e.matmul(pb[:, 0:NCOL], lhsT=Wup[:, k, ht * 128:(ht + 1) * 128], rhs=hnT[:, k, :], start=(k == 0), stop=(k == 7)), reads=[Wup, hnT], writes=[pb])
                rr = rl[ht % 2]
                op("act", lambda e, pb=pb, rr=rr: e.activation(out=rr[:], in_=pb[:, 0:NCOL], func=AF.Relu), reads=[pb], writes=[rr])
                op("pool", lambda e, rr=rr, ht=ht: e.tensor_tensor(out=hid[:, ht, :], in0=rr[:], in1=rr[:], op=ALU.mult), reads=[rr], writes=[hid])
            for ct in range(8):
                pb = bank()
                for k in range(32):
                    op("pe", lambda e, pb=pb, k=k, ct=ct: e.matmul(pb[:, 0:NCOL], lhsT=Wdn[:, k, ct * 128:(ct + 1) * 128], rhs=hid[:, k, :], start=(k == 0), stop=(k == 31)), reads=[Wdn, hid], writes=[pb])
                op("act", lambda e, pb=pb, ct=ct: e.activation(out=ffT[:, ct, :], in_=pb[:, 0:NCOL], func=AF.Copy), reads=[pb], writes=[ffT])
            for tt in range(2):
                for kh in range(2):
                    pb = bank()
                    for k4 in range(4):
                        k = kh * 4 + k4
                        op("pe", lambda e, pb=pb, k=k, k4=k4, tt=tt: e.transpose(pb[:, k4 * 128:(k4 + 1) * 128], ffT[:, k, tt * 128:(tt + 1) * 128], ident), reads=[ffT, PB], writes=[pb])
                    op("dve", lambda e, pb=pb, kh=kh: e.tensor_copy(out=tokB[:, kh * 512:(kh + 1) * 512], in_=pb[:, :]), reads=[pb], writes=[tokB])
                op("dve", lambda e, tt=tt: e.scalar_tensor_tensor(out=tokA[:], in0=tokB[:], scalar=1.0, in1=tokB[:], op0=ALU.mult, op1=ALU.mult, accum_out=sst[:, 2 + tt:3 + tt]), reads=[tokB], writes=[tokA, sst])
                rstd_from_ss2(2 + tt)
                op("dve", lambda e, tt=tt: e.scalar_tensor_tensor(out=tokA[:], in0=tokB[:], scalar=sst[:, 2 + tt:3 + tt], in1=P2("gBmlp"), op0=ALU.mult, op1=ALU.mult), reads=[tokB, sst, PB], writes=[tokA])
                op("dve", lambda e, tt=tt: e.tensor_tensor(out=tokA[:], in0=tokA[:], in1=htok[tt][:], op=ALU.add), reads=[tokA, htok[tt]], writes=[tokA])
                for b2 in range(2):
                    dma("sp", None, out[2 * tt + b2, t0:t0 + TC, :], tokA[64 * b2:64 * b2 + 64, :], reads=[tokA], out_final=True)
        fw2.emit()
    fw.stack.close()
    return nc


_CACHE = {}


def kernel(**inputs):
    inp = {k: np.asarray(v) for k, v in inputs.items()}
    A, B = build_params(inp)
    ppa, ppb = A.pack(), B.pack()
    key = "full"
    if key not in _CACHE:
        _CACHE[key] = build_program(A.off, A.n, B.off, B.n)
    nc = _CACHE[key]
    x = np.ascontiguousarray(inp["x"], dtype=np.float32)
    sq = lambda k: np.ascontiguousarray(inp[k][0], dtype=np.float32)
    shared = {"ppa": ppa, "ppb": ppb, "w_in": sq("w_in"), "w_out": sq("w_out"), "w_glu": sq("s5_w_glu"),
              "w_ff_up": sq("w_ff_up"), "w_ff_down": sq("w_ff_down")}
    in_maps = [dict(shared, x=np.ascontiguousarray(x[c * NB:(c + 1) * NB])) for c in range(NCORES)]
    res = run_bass_kernel_spmd(nc, in_maps, core_ids=list(range(NCORES)))
    return np.concatenate([r["out"] for r in res.results], axis=0)
```

```python
import contextlib
import types
import numpy as np
import concourse.bass as bass
import concourse.mybir as mybir
from concourse.bass_utils import run_bass_kernel_spmd

F32 = mybir.dt.float32
BF16 = mybir.dt.bfloat16
AF = mybir.ActivationFunctionType
ALU = mybir.AluOpType


def _freeze(fn):
    if fn.__closure__ is None:
        return fn
    cells = []
    for c in fn.__closure__:
        try:
            cells.append(types.CellType(c.cell_contents))
        except ValueError:
            cells.append(c)
    g = types.FunctionType(fn.__code__, fn.__globals__, fn.__name__, fn.__defaults__, tuple(cells))
    g.__kwdefaults__ = fn.__kwdefaults__
    return g


class Buf:
    def __init__(self, fw, name, t):
        self.fw = fw
        self.name = name
        self.t = t
        self.last_w = None
        self.readers = []
        self.ld_sem = None
        self.ld_cnt = 0
        self.st_sem = None
        self.st_cnt = 0

    def __getitem__(self, k):
        return self.t[k]


class FW:
    ENG = ("pe", "act", "dve", "pool", "sp")

    def __init__(self, nc):
        self.nc = nc
        self.stack = contextlib.ExitStack()
        self.tstack = contextlib.ExitStack()
        self.eng_obj = {"pe": nc.tensor, "act": nc.scalar, "dve": nc.vector,
                        "pool": nc.gpsimd, "sp": nc.sync}
        self.sem = {}
        self.cnt = {e: 0 for e in self.ENG}
        self.prog = {e: [] for e in self.ENG}
        self.waited = {e: {} for e in self.ENG}
        self.final_tokens = []
        self.all_sems = []
        self.same_engine_sync = True
        self.nsem = 0

    def scope(self):
        return self.stack

    def tscope(self):
        self.tstack = contextlib.ExitStack()
        return self.tstack

    def new_sem(self, name):
        self.nsem += 1
        s = self.stack.enter_context(self.nc.semaphore(f"{name}_{self.nsem}"))
        self.all_sems.append(s)
        return s

    def init_sems(self):
        for e in self.ENG:
            if e not in self.sem:
                self.sem[e] = self.new_sem("s_" + e)

    NOZERO = ("PA", "PB", "Win", "Wup", "Wdn")

    def sb(self, name, shape, dtype):
        t = self.tstack.enter_context(self.nc.sbuf_tensor(name, list(shape), dtype))
        b = Buf(self, name, t)
        if name not in self.NOZERO:
            self.op("pool", lambda e: e.memset(b[:], 0.0), writes=[b])
        return b

    def ps(self, name, shape, dtype):
        t = self.tstack.enter_context(self.nc.psum_tensor(name, list(shape), dtype))
        b = Buf(self, name, t)
        self.op("dve", lambda e: e.memset(b[:], 0.0), writes=[b])
        return b

    def view(self, name, ap):
        return Buf(self, name, ap)

    def _deps(self, reads, writes):
        deps = []
        for b in reads:
            if b.last_w is not None:
                deps.append(b.last_w)
        for b in writes:
            if b.last_w is not None:
                deps.append(b.last_w)
            deps.extend(b.readers)
        return deps

    def _emit_waits(self, eng, deps):
        w = self.waited[eng]
        for (sem, val, src) in deps:
            if src == eng and (eng == "pe" or not self.same_engine_sync):
                continue
            key = id(sem)
            if w.get(key, 0) >= val:
                continue
            w[key] = val
            self.prog[eng].append(("wait", sem, val))

    def op(self, eng, fn, reads=(), writes=()):
        self.init_sems()
        reads = [b for b in reads if b is not None]
        writes = [b for b in writes if b is not None]
        self._emit_waits(eng, self._deps(reads, writes))
        self.cnt[eng] += 1
        tok = (self.sem[eng], self.cnt[eng], eng)
        self.prog[eng].append(("op", _freeze(fn), self.sem[eng], 1))
        for b in reads:
            b.readers.append(tok)
        for b in writes:
            b.last_w = tok
            b.readers = []
        return tok

    def dma(self, q, dst, out_ap, in_ap, reads=(), out_final=False, **kw):
        self.init_sems()
        reads = [b for b in reads if b is not None]
        writes = [dst] if dst is not None else []
        self._emit_waits(q, self._deps(reads, writes))
        if dst is not None:
            if dst.ld_sem is None:
                dst.ld_sem = self.new_sem("ld_" + dst.name)
            dst.ld_cnt += 16
            tok = (dst.ld_sem, dst.ld_cnt, "dma")
            sem = dst.ld_sem
            dst.last_w = tok
            dst.readers = []
            for b in reads:
                b.readers.append(tok)
        else:
            src = reads[0]
            if src.st_sem is None:
                src.st_sem = self.new_sem("st_" + src.name)
            src.st_cnt += 16
            tok = (src.st_sem, src.st_cnt, "dma")
            sem = src.st_sem
            for b in reads:
                b.readers.append(tok)
            if out_final:
                self.final_tokens.append(tok)
        fn = (lambda e, o=out_ap, i=in_ap, k=kw: e.dma_start(out=o, in_=i, **k))
        self.prog[q].append(("op", fn, sem, 16))
        return tok

    def drain_all(self):
        self.init_sems()
        deps = [(self.sem[e], self.cnt[e], e) for e in self.ENG if e != "sp" and self.cnt[e] > 0]
        deps += self.final_tokens
        self._emit_waits("sp", deps)

    def emit(self):
        self.drain_all()
        nc = self.nc
        with nc.Block() as block:
            def mk(eng):
                def body(e):
                    for item in self.prog[eng]:
                        if item[0] == "wait":
                            e.wait_ge(item[1], item[2])
                        else:
                            ins = item[1](e)
                            ins.then_inc(item[2], item[3])
                return body
            block.tensor(mk("pe"))
            block.scalar(mk("act"))
            block.vector(mk("dve"))
            block.gpsimd(mk("pool"))
            block.sync(mk("sp"))
        self.prog = {e: [] for e in self.ENG}


NCORES = 8
NB = 4
SEQ = 2048
D = 1024
TC = 64
NIT = SEQ // TC
NCOL = NB * TC
DS5 = 512
NPROJ = 2304
DFF = 4096
SUB = 16
GN_EPS = 64e-5


class PP:
    def __init__(self):
        self.off = {}
        self.n = 0
        self.parts = []

    def add(self, name, arr):
        arr = np.ascontiguousarray(arr, dtype=np.float32)
        if arr.shape[0] < 128:
            pad = np.zeros((128 - arr.shape[0],) + arr.shape[1:], np.float32)
            arr = np.concatenate([arr, pad], 0)
        arr = arr.reshape(128, -1)
        self.off[name] = (self.n, arr.shape[1])
        self.n += arr.shape[1]
        self.parts.append(arr)

    def pack(self):
        return np.ascontiguousarray(np.concatenate(self.parts, 1))


def pk(v, nt):
    return np.asarray(v, np.float32).reshape(nt, 128).T


def build_params(inp):
    g = lambda k: np.asarray(inp[k], np.float32)[0]
    A = PP()
    A.add("gpre", pk(g("g_pre_mix"), 8))
    s5l = lambda a: a.reshape(16, 2, 64).transpose(1, 2, 0).reshape(128, 16)
    A.add("lamre", s5l(g("s5_lam_re")))
    A.add("lamim", s5l(g("s5_lam_im")))
    A.add("logdt", s5l(np.repeat(g("s5_log_dt")[:, None], 64, 1)))
    s5b = lambda a: a.reshape(16, 2, 64, 16).transpose(1, 2, 0, 3).reshape(128, 16 * 16)
    A.add("bre", s5b(g("s5_b_re")))
    A.add("bim", s5b(g("s5_b_im")))

    def ctl(c):
        o = np.zeros((2, 64, 16, 2, 16), np.float32)
        cc = c.reshape(16, 2, 16, 64)
        for g2 in range(2):
            o[g2, :, :, g2, :] = cc[:, g2].transpose(2, 0, 1)
        return o.reshape(128, 16 * 32)
    A.add("ctre", ctl(g("s5_c_re")))
    A.add("ctim", ctl(g("s5_c_im")))
    def pk96(v):
        o = np.zeros((128, 6), np.float32)
        for tl in range(6):
            n = 96 if tl < 5 else 32
            o[:n, tl] = v[96 * tl:96 * tl + n]
        return o
    A.add("dskip", pk96(g("s5_d")))
    A.add("bglu", pk96(g("s5_b_glu")))
    A.add("mu", pk(g("rw_mu"), 14))
    A.add("w0", pk(g("rw_w0"), 4))
    A.add("a0", pk(g("rw_a0"), 4))
    A.add("kk", pk(g("rw_k_k"), 4))
    A.add("ka", pk(g("rw_k_a"), 4))
    A.add("rk", pk(g("rw_r_k").reshape(-1), 4))
    A.add("gnw", pk(g("rw_gn_w"), 4))
    A.add("gnb", pk(g("rw_gn_b"), 4))
    A.add("wau", np.concatenate([g("rw_w_up"), g("rw_a_up")], 0))
    A.add("gup", g("rw_g_up"))
    A.add("gBmix", np.repeat(g("g_post_mix")[None, :], 128, 0))
    A.add("ident", np.eye(128, dtype=np.float32))
    ob = np.zeros((128, 128), np.float32)
    ob[:64, :64] = 1
    ob[64:, 64:] = 1
    A.add("onesblk", ob)
    j = np.arange(64)[:, None]
    t = np.arange(64)[None, :]
    mg = np.concatenate([(t > j), (t >= j)], 1).astype(np.float32)
    A.add("maskG", np.concatenate([mg, mg], 0))
    A.add("maskL", np.concatenate([(j > t), (j > t)], 0).astype(np.float32))
    sm = np.ones((128, NB, TC), np.float32)
    sm[:, :, 0] = 0
    A.add("scanmask", sm.reshape(128, NCOL))
    B = PP()
    B.add("gpremlp", pk(g("g_pre_mlp"), 8))
    B.add("gBmlp", np.repeat(g("g_post_mlp")[None, :], 128, 0))
    B.add("ident", np.eye(128, dtype=np.float32))
    return A, B


def build_program(offA, nA, offB, nB, nit=NIT, do_ffn=True):
    nc = bass.Bass("TRN2", target_bir_lowering=False)
    x = nc.dram_tensor("x", [NB, SEQ, D], F32, kind="ExternalInput").ap()
    ppa = nc.dram_tensor("ppa", [128, nA], F32, kind="ExternalInput").ap()
    ppb = nc.dram_tensor("ppb", [128, nB], F32, kind="ExternalInput").ap()
    w_in = nc.dram_tensor("w_in", [D, NPROJ], F32, kind="ExternalInput").ap()
    w_out = nc.dram_tensor("w_out", [D, D], F32, kind="ExternalInput").ap()
    w_glu = nc.dram_tensor("w_glu", [DS5, DS5], F32, kind="ExternalInput").ap()
    w_up = nc.dram_tensor("w_ff_up", [D, DFF], F32, kind="ExternalInput").ap()
    w_dn = nc.dram_tensor("w_ff_down", [DFF, D], F32, kind="ExternalInput").ap()
    out = nc.dram_tensor("out", [NB, SEQ, D], F32, kind="ExternalOutput").ap()
    hsc = nc.dram_tensor("hsc", [NB, SEQ, D], F32, kind="ExternalOutput").ap()

    import os
    SKIP = os.environ.get("KSKIP", "")
    TAPS = set(os.environ.get("KTAPS", "").split(",")) - {""}
    KSTOP = int(os.environ.get("KSTOP", "99"))
    SCAN_ENG = os.environ.get("KSCAN", "pool")
    fw = FW(nc)
    tapped = set()

    def tap(name, buf, ap):
        if name not in TAPS or name in tapped:
            return
        tapped.add(name)
        dt_ = ap.dtype
        dd = nc.dram_tensor("dbg_" + name, list(ap.shape), dt_, kind="ExternalOutput").ap()
        fw.dma("sp", None, dd, ap, reads=[buf], out_final=True)
    fw.stack.__enter__()
    with fw.tscope():
        sb, op, dma = fw.sb, fw.op, fw.dma
        PA = sb("PA", [128, nA], F32)

        def P(name, c0=0, c1=None, rows=slice(0, 128)):
            o, n = offA[name]
            c1 = n if c1 is None else c1
            return PA[rows, o + c0:o + c1]
        Win = sb("Win", [128, 8, NPROJ], BF16)
        Wout = sb("Wout", [128, 10, D], BF16)
        Wglu = sb("Wglu", [128, 6, DS5], BF16)
        TR = [96, 96, 96, 96, 96, 32]
        banks = [fw.ps(f"bank{i}", [128, 512], F32) for i in range(8)]
        bi = [0]

        def bank():
            b = banks[bi[0] % 8]
            bi[0] += 1
            return b

        dma("sp", PA, PA[:], ppa)
        for k in range(8):
            for h in range(2):
                dma("pool", Win, Win[:, k, h * 1152:(h + 1) * 1152], w_in[k * 128:(k + 1) * 128, h * 1152:(h + 1) * 1152])
        for k in range(6):
            dma("pool", Wout, Wout[0:TR[k], k, :], w_out[k * 96:k * 96 + TR[k], :])
            dma("pool", Wglu, Wglu[0:TR[k], k, :], w_glu[k * 96:k * 96 + TR[k], :])
        for k in range(4):
            dma("pool", Wout, Wout[:, 6 + k, :], w_out[512 + k * 128:512 + (k + 1) * 128, :])

        s5t = sb("s5t", [128, 12, 16], F32)
        T = lambda i: s5t[:, i, :]
        AR, AI, FR, FI = 0, 1, 2, 3
        tt_ = lambda o, a, b, f: op("dve", lambda e: e.tensor_tensor(out=o, in0=a, in1=b, op=f), reads=[PA, s5t], writes=[s5t])
        ts_ = lambda o, a, s1, s2, f1, f2: op("dve", lambda e: e.tensor_scalar(out=o, in0=a, scalar1=s1, scalar2=s2, op0=f1, op1=f2), reads=[PA, s5t], writes=[s5t])
        ac_ = lambda o, a, f, **kw: op("act", lambda e: e.activation(out=o, in_=a, func=f, **kw), reads=[PA, s5t], writes=[s5t])
        ac_(T(4), P("logdt"), AF.Exp)
        tt_(T(5), P("lamre"), T(4), ALU.mult)
        tt_(T(6), P("lamim"), T(4), ALU.mult)
        ac_(T(7), T(5), AF.Exp)
        ts_(T(8), T(6), 1.0 / 16, None, ALU.mult, ALU.bypass)
        ts_(T(9), T(6), 1.0 / 16, float(np.pi / 2), ALU.mult, ALU.add)
        ac_(T(8), T(8), AF.Sin)
        ac_(T(9), T(9), AF.Sin)
        for _ in range(4):
            tt_(T(10), T(8), T(9), ALU.mult)
            tt_(T(11), T(8), T(8), ALU.mult)
            tt_(T(9), T(9), T(9), ALU.mult)
            tt_(T(9), T(9), T(11), ALU.subtract)
            ts_(T(8), T(10), 2.0, None, ALU.mult, ALU.bypass)
        tt_(T(AR), T(7), T(9), ALU.mult)
        tt_(T(AI), T(7), T(8), ALU.mult)
        tt_(T(4), P("lamre"), P("lamre"), ALU.mult)
        tt_(T(5), P("lamim"), P("lamim"), ALU.mult)
        tt_(T(4), T(4), T(5), ALU.add)
        op("dve", lambda e: e.reciprocal(out=T(4), in_=T(4)), reads=[s5t], writes=[s5t])
        ts_(T(5), T(AR), -1.0, None, ALU.add, ALU.bypass)
        tt_(T(6), T(5), P("lamre"), ALU.mult)
        tt_(T(7), T(AI), P("lamim"), ALU.mult)
        tt_(T(6), T(6), T(7), ALU.add)
        tt_(T(FR), T(6), T(4), ALU.mult)
        tt_(T(6), T(AI), P("lamre"), ALU.mult)
        tt_(T(7), T(5), P("lamim"), ALU.mult)
        tt_(T(6), T(6), T(7), ALU.subtract)
        tt_(T(FI), T(6), T(4), ALU.mult)
        Acf = sb("Acf", [128, 2, 2, 16], F32)
        for c in range(2):
            op("dve", lambda e, c=c: e.tensor_copy(out=Acf[:, 0, c, :], in_=T(AR)), reads=[s5t], writes=[Acf])
        op("dve", lambda e: e.tensor_copy(out=Acf[:, 1, 0, :], in_=T(AI)), reads=[s5t], writes=[Acf])
        op("dve", lambda e: e.tensor_scalar(out=Acf[:, 1, 1, :], in0=T(AI), scalar1=-1.0, scalar2=None, op0=ALU.mult), reads=[s5t], writes=[Acf])
        tap("Acf", Acf, Acf[:])
        tap("s5t", s5t, s5t[:])
        BD = sb("BD", [128, 2, 96], F32)
        BT = sb("BT", [128, 6, 2, 128], BF16)
        bb1 = sb("bb1", [128, 16], F32)
        bb2 = sb("bb2", [128, 16], F32)
        o_bre, o_bim = offA["bre"][0], offA["bim"][0]
        for gp in range(16):
            bre = PA[:, o_bre + gp * 16:o_bre + gp * 16 + 16]
            bim = PA[:, o_bim + gp * 16:o_bim + gp * 16 + 16]
            fr, fi = s5t[:, FR, gp:gp + 1], s5t[:, FI, gp:gp + 1]
            for g2 in range(2):
                rs = slice(64 * g2, 64 * g2 + 64)
                cs = slice(32 * (gp % 3) + 16 * g2, 32 * (gp % 3) + 16 * g2 + 16)
                op("dve", lambda e, rs=rs, bim=bim, fi=fi: e.tensor_scalar(out=bb1[rs, :], in0=bim[rs, :], scalar1=fi[rs, :], scalar2=None, op0=ALU.mult), reads=[PA, s5t], writes=[bb1])
                op("dve", lambda e, rs=rs, cs=cs, bre=bre, fr=fr: e.scalar_tensor_tensor(out=BD[rs, 0, cs], in0=bre[rs, :], scalar=fr[rs, :], in1=bb1[rs, :], op0=ALU.mult, op1=ALU.subtract), reads=[PA, s5t, bb1], writes=[BD])
                op("dve", lambda e, rs=rs, bre=bre, fi=fi: e.tensor_scalar(out=bb2[rs, :], in0=bre[rs, :], scalar1=fi[rs, :], scalar2=-1.0, op0=ALU.mult, op1=ALU.mult), reads=[PA, s5t], writes=[bb2])
                op("dve", lambda e, rs=rs, cs=cs, bim=bim, fr=fr: e.scalar_tensor_tensor(out=bb1[rs, :], in0=bim[rs, :], scalar=fr[rs, :], in1=bb2[rs, :], op0=ALU.mult, op1=ALU.subtract), reads=[PA, s5t, bb2], writes=[bb1])
                op("dve", lambda e, rs=rs, cs=cs: e.tensor_scalar(out=BD[rs, 1, cs], in0=bb1[rs, :], scalar1=-1.0, scalar2=None, op0=ALU.mult), reads=[bb1], writes=[BD])
            if gp % 3 == 2 or gp == 15:
                tl = gp // 3
                m = 96 if tl < 5 else 32
                pb = bank()
                for w_ in range(2):
                    op("pe", lambda e, pb=pb, w_=w_, m=m: e.transpose(pb[0:m, w_ * 128:(w_ + 1) * 128], BD[:, w_, 0:m], P("ident")), reads=[BD, PA], writes=[pb])
                op("act", lambda e, pb=pb, tl=tl, m=m: e.activation(out=BT[0:m, tl, :, :], in_=pb[0:m, 0:256].rearrange("p (w c) -> p w c", w=2), func=AF.Copy), reads=[pb], writes=[BT])

        tap("BT", BT, BT[:])
        xtok = [sb(f"xtok{i}", [128, D], F32) for i in range(2)]
        tokA = sb("tokA", [128, D], F32)
        tokB = sb("tokB", [128, D], F32)
        sst = sb("sst", [128, 8], F32)
        xnT = sb("xnT", [128, 8, NCOL], BF16)
        Ub = sb("Ub", [128, 6, NB, TC], BF16)
        Sb = sb("Sb", [128, 2, 16, NB, SUB], BF16)
        CTb = sb("CTb", [128, 2, 512], BF16)
        op("act", lambda e: e.activation(out=CTb[:, 0, :], in_=P("ctre"), func=AF.Copy), reads=[PA], writes=[CTb])
        op("act", lambda e: e.activation(out=CTb[:, 1, :], in_=P("ctim"), func=AF.Copy), reads=[PA], writes=[CTb])
        QB = [sb(f"qb{i}", [128, NB, TC + 1], F32) for i in range(14)]
        S = sb("S", [128, 2, 16, NB, SUB + 1], F32)
        stmp = sb("stmp", [128, 2, 2, 16, NB], F32)
        Y = sb("Y", [128, 6, NB, SUB], F32)
        Y1 = sb("Y1", [128, 6, NB, SUB], F32)
        Y2 = Y1
        ZbF = sb("ZbF", [128, 6, NB, TC], BF16)
        SGf = sb("SGf", [128, NB, TC], F32)
        mixin = sb("mixin", [128, 10, NB, TC], BF16)
        mixT = sb("mixT", [128, 8, NCOL], F32)
        ST = [sb(f"ST{j}", [128, NB, 64], F32) for j in range(4)]
        XWA = sb("XWA", [128, NCOL], F32)
        TH = sb("TH", [128, NCOL], F32)
        SGX = sb("SGX", [128, NCOL], F32)
        names = ["qr", "qk", "qv", "sgw", "aa", "gg", "kkt", "t1", "t2", "kap", "kp", "bet", "bon",
                 "cum", "Pm", "Pinv", "oT", "cen"]
        R_ = {n: sb("r_" + n, [128, NCOL], F32) for n in names}
        R_["dd"] = R_["t2"]
        KR = sb("KR", [128, 2, NCOL], BF16)
        OUTS = ["Pm", "bon", "gg"]
        KRs = [KR, sb("KR1", [128, 2, NCOL], BF16)]
        identb = sb("identb", [128, 128], BF16)
        op("act", lambda e: e.activation(out=identb[:], in_=P("ident"), func=AF.Copy), reads=[PA], writes=[identb])
        STb = [sb(f"STb{j}", [128, NB, 64], BF16) for j in range(4)]
        RS13, RS48 = [], []
        t1b = sb("r_t1b", [128, NCOL], F32)
        t2b = sb("r_t2b", [128, NCOL], F32)
        for par in range(2):
            d13 = dict(R_)
            if par == 1:
                for n_ in OUTS:
                    d13[n_] = sb("r1_" + n_, [128, NCOL], F32)
            d13["lw"] = d13["sgw"]
            d13["Ppv"] = d13["t2"]
            d13["bt"] = sb(f"r{par}_btb", [128, NCOL], BF16)
            d13["kt"] = sb(f"r{par}_ktb", [128, NCOL], BF16)
            d13["qvb"] = sb(f"r{par}_qvb", [128, NCOL], BF16)
            RS13.append(d13)
            d48 = {n_: d13[n_] for n_ in OUTS}
            d48["bt"] = d13["bt"]
            d48["kt"] = d13["kt"]
            d48["qvb"] = d13["qvb"]
            d48["oT"] = R_["oT"]
            d48["cen"] = R_["cen"]
            d48["t1"] = t1b
            d48["t2"] = t2b
            RS48.append(d48)
        Vt = sb("Vt", [128, NB, 64], BF16)
        Btk = sb("Btk", [128, NB, 64], BF16)
        Ktk = sb("Ktk", [128, NB, 64], BF16)
        GbS = sb("GbS", [128, NB, 128], BF16)
        GkS = sb("GkS", [128, NB, 128], BF16)
        Xa = [sb(f"Xa{i}", [128, NB, 64], BF16) for i in range(2)]
        XTa = [sb(f"XTa{i}", [128, NB, 64], BF16) for i in range(2)]
        Wt = sb("Wt", [128, NB, 64], F32)
        Wtb = sb("Wtb", [128, NB, 64], BF16)
        Rn = sb("Rn", [128, NB, 64], BF16)
        Uu = sb("Uu", [128, NB, 64], BF16)
        omka = sb("omka", [128, 4], F32)

        op("dve", lambda e: e.tensor_scalar(out=omka[:], in0=P("ka"), scalar1=-1.0, scalar2=1.0, op0=ALU.mult, op1=ALU.add), reads=[PA], writes=[omka])

        def rstd_from_ss(col):
            op("dve", lambda e: e.tensor_scalar(out=sst[:, col:col + 1], in0=sst[:, col:col + 1], scalar1=1.0 / D, scalar2=1e-6, op0=ALU.mult, op1=ALU.add), reads=[sst], writes=[sst])
            op("act", lambda e: e.activation(out=sst[:, col:col + 1], in_=sst[:, col:col + 1], func=AF.Sqrt), reads=[sst], writes=[sst])
            op("dve", lambda e: e.reciprocal(out=sst[:, col:col + 1], in_=sst[:, col:col + 1]), reads=[sst], writes=[sst])

        ident = P("ident")
        onesblk = P("onesblk")

        for it in range(nit):
            t0 = it * TC
            for tt in range(2):
                for b2 in range(2):
                    dma("sp", xtok[tt], xtok[tt][64 * b2:64 * b2 + 64, :], x[2 * tt + b2, t0:t0 + TC, :])
            for tt in range(2):
                op("dve", lambda e, tt=tt: e.scalar_tensor_tensor(out=tokA[:], in0=xtok[tt][:], scalar=1.0, in1=xtok[tt][:], op0=ALU.mult, op1=ALU.mult, accum_out=sst[:, tt:tt + 1]), reads=[xtok[tt]], writes=[tokA, sst])
                rstd_from_ss(tt)
                op("dve", lambda e, tt=tt: e.tensor_scalar(out=tokA[:], in0=xtok[tt][:], scalar1=sst[:, tt:tt + 1], scalar2=None, op0=ALU.mult), reads=[xtok[tt], sst], writes=[tokA])
                for kh in range(2):
                    pb = bank()
                    for k4 in range(4):
                        k = kh * 4 + k4
                        op("pe", lambda e, pb=pb, k=k, k4=k4: e.transpose(pb[:, k4 * 128:(k4 + 1) * 128], tokA[:, k * 128:(k + 1) * 128], ident), reads=[tokA, PA], writes=[pb])
                    gpre = P("gpre", kh * 4, kh * 4 + 4).unsqueeze(2).to_broadcast([128, 4, 128])
                    op("dve", lambda e, pb=pb, kh=kh, tt=tt, gpre=gpre: e.tensor_tensor(out=xnT[:, kh * 4:kh * 4 + 4, tt * 128:(tt + 1) * 128], in0=pb[:, :].rearrange("p (k c) -> p k c", k=4), in1=gpre, op=ALU.mult), reads=[pb, PA], writes=[xnT])
            for ct in range(20):
                pb = bank()
                if ct < 6:
                    c0, m = 96 * ct, TR[ct]
                else:
                    c0, m = 512 + 128 * (ct - 6), 128
                for k in range(8):
                    op("pe", lambda e, pb=pb, k=k, c0=c0, m=m: e.matmul(pb[0:m, 0:NCOL], lhsT=Win[:, k, c0:c0 + m], rhs=xnT[:, k, :], start=(k == 0), stop=(k == 7)), reads=[Win, xnT], writes=[pb])
                if ct < 6:
                    op("act", lambda e, pb=pb, ct=ct, m=m: e.activation(out=Ub[0:m, ct, :, :], in_=pb[0:m, 0:NCOL].rearrange("p (b t) -> p b t", b=NB), func=AF.Copy), reads=[pb], writes=[Ub])
                else:
                    qb = QB[ct - 6]
                    op("act", lambda e, pb=pb, qb=qb: e.activation(out=qb[:, :, 1:TC + 1], in_=pb[:, 0:NCOL].rearrange("p (b t) -> p b t", b=NB), func=AF.Copy), reads=[pb], writes=[qb])

            def s5_front(sc):
                tsl = slice(sc * SUB, (sc + 1) * SUB)
                for gp in range(16):
                    tl, q = gp // 3, gp % 3
                    pb = bank()
                    for w_ in range(2):
                        op("pe", lambda e, pb=pb, tl=tl, q=q, w_=w_: e.matmul(pb[:, w_ * NB * SUB:(w_ + 1) * NB * SUB], lhsT=BT[32 * q:32 * q + 32, tl, w_, :], rhs=Ub[32 * q:32 * q + 32, tl, :, tsl], start=True, stop=True), reads=[BT, Ub], writes=[pb])
                    op("act", lambda e, pb=pb, gp=gp: e.activation(out=S[:, :, gp, :, 1:SUB + 1], in_=pb[:, 0:2 * NB * SUB].rearrange("p (c b t) -> p c b t", c=2, b=NB), func=AF.Copy), reads=[pb], writes=[S])
                for t in range(SUB):
                    prev = S[:, :, :, :, t]
                    cur = S[:, :, :, :, t + 1]
                    a1 = Acf[:, 0, :, :].unsqueeze(3).to_broadcast([128, 2, 16, NB])
                    op(SCAN_ENG, lambda e, prev=prev, a1=a1: e.tensor_tensor(out=stmp[:, 0, :, :, :], in0=prev, in1=a1, op=ALU.mult), reads=[S, Acf], writes=[stmp])
                    for c in range(2):
                        a2 = Acf[:, 1, c, :].unsqueeze(2).to_broadcast([128, 16, NB])
                        op(SCAN_ENG, lambda e, c=c, a2=a2, t=t: e.tensor_tensor(out=stmp[:, 1, c, :, :], in0=S[:, 1 - c, :, :, t], in1=a2, op=ALU.mult), reads=[S, Acf], writes=[stmp])
                    op(SCAN_ENG, lambda e: e.tensor_tensor(out=stmp[:, 0, :, :, :], in0=stmp[:, 0, :, :, :], in1=stmp[:, 1, :, :, :], op=ALU.add), reads=[stmp], writes=[stmp])
                    op(SCAN_ENG, lambda e, cur=cur: e.tensor_tensor(out=cur, in0=cur, in1=stmp[:, 0, :, :, :], op=ALU.add), reads=[stmp, S], writes=[S])
                tap("S", S, S[:])

            def s5_back(sc):
                tsl = slice(sc * SUB, (sc + 1) * SUB)
                op("act", lambda e: e.activation(out=Sb[:, :, :, :, :], in_=S[:, :, :, :, 1:SUB + 1], func=AF.Copy), reads=[S], writes=[Sb])
                for tl in range(6):
                    pb = bank()
                    for q in range(3 if tl < 5 else 1):
                        gp = tl * 3 + q
                        oc, nct = offA["ctre"][0], offA["ctim"][0]
                        op("pe", lambda e, pb=pb, q=q, gp=gp, oc=oc: e.matmul(pb[32 * q:32 * q + 32, 0:NB * SUB], lhsT=CTb[:, 0, gp * 32:gp * 32 + 32], rhs=Sb[:, 0, gp, :, :], start=True, stop=False), reads=[CTb, Sb], writes=[pb])
                        op("pe", lambda e, pb=pb, q=q, gp=gp, nct=nct: e.matmul(pb[32 * q:32 * q + 32, 0:NB * SUB], lhsT=CTb[:, 1, gp * 32:gp * 32 + 32], rhs=Sb[:, 1, gp, :, :], start=False, stop=True), reads=[CTb, Sb], writes=[pb])
                    op("dve", lambda e, pb=pb, tl=tl: e.scalar_tensor_tensor(out=Y[:, tl, :, :], in0=Ub[:, tl, :, tsl], scalar=P("dskip", tl, tl + 1), in1=pb[:, 0:NB * SUB].rearrange("p (b t) -> p b t", b=NB), op0=ALU.mult, op1=ALU.add), reads=[Ub, PA, pb], writes=[Y])
                tap("Y", Y, Y[:])
                op(SCAN_ENG, lambda e: e.tensor_copy(out=S[:, :, :, :, 0], in_=S[:, :, :, :, SUB]), reads=[S], writes=[S])
                fl = lambda b_: b_[:].rearrange("p a b t -> p (a b t)")
                op("dve", lambda e: e.tensor_tensor(out=fl(Y1), in0=fl(Y), in1=fl(Y), op=ALU.mult), reads=[Y], writes=[Y1])
                op("dve", lambda e: e.tensor_scalar(out=fl(Y1), in0=fl(Y1), scalar1=0.044715, scalar2=1.0, op0=ALU.mult, op1=ALU.add), reads=[Y1], writes=[Y1])
                op("dve", lambda e: e.tensor_tensor(out=fl(Y1), in0=fl(Y1), in1=fl(Y), op=ALU.mult), reads=[Y, Y1], writes=[Y1])
                op("act", lambda e: e.activation(out=fl(Y2), in_=fl(Y1), func=AF.Sigmoid, scale=1.5957691216057308), reads=[Y1], writes=[Y2])
                op("dve", lambda e: e.tensor_tensor(out=ZbF[:, :, :, tsl], in0=Y[:, :, :, :], in1=Y2[:, :, :, :], op=ALU.mult), reads=[Y, Y2], writes=[ZbF])
                if sc != TC // SUB - 1:
                    return
                for ct in range(6):
                    pb = bank()
                    m = TR[ct]
                    for k in range(6):
                        op("pe", lambda e, pb=pb, k=k, ct=ct, m=m: e.matmul(pb[0:m, 0:NCOL], lhsT=Wglu[0:TR[k], k, ct * 96:ct * 96 + m], rhs=ZbF[0:TR[k], k, :, :], start=(k == 0), stop=(k == 5)), reads=[Wglu, ZbF], writes=[pb])
                    op("act", lambda e, pb=pb, ct=ct, m=m: e.activation(out=SGf[0:m, :, :], in_=pb[0:m, 0:NCOL].rearrange("p (b t) -> p b t", b=NB), func=AF.Sigmoid, bias=P("bglu", ct, ct + 1, rows=slice(0, m))), reads=[pb, PA], writes=[SGf])
                    op("dve", lambda e, ct=ct, m=m: e.tensor_tensor(out=mixin[0:m, ct, :, :], in0=ZbF[0:m, ct, :, :], in1=SGf[0:m, :, :], op=ALU.mult), reads=[ZbF, SGf], writes=[mixin])

            def shift(dst, qb, mucol):
                v3 = lambda b_: b_[:].rearrange("p (b t) -> p b t", b=NB)
                op("dve", lambda e: e.tensor_tensor(out=v3(R_["dd"]), in0=qb[:, :, 0:TC], in1=qb[:, :, 1:TC + 1], op=ALU.subtract), reads=[qb], writes=[R_["dd"]])
                op("dve", lambda e: e.scalar_tensor_tensor(out=v3(dst), in0=v3(R_["dd"]), scalar=P("mu", mucol, mucol + 1), in1=qb[:, :, 1:TC + 1], op0=ALU.mult, op1=ALU.add), reads=[R_["dd"], qb, PA], writes=[dst])
                op("dve", lambda e: e.tensor_copy(out=qb[:, :, 0:1], in_=qb[:, :, TC:TC + 1]), reads=[qb], writes=[qb])

            shift(XWA, QB[12], 12)
            op("act", lambda e: e.activation(out=TH[0:64, :], in_=XWA[0:64, :], func=AF.Tanh), reads=[XWA], writes=[TH])
            shift(SGX, QB[13], 13)
            op("act", lambda e: e.activation(out=SGX[:], in_=SGX[:], func=AF.Sigmoid), reads=[SGX], writes=[SGX])
            tt2 = lambda o, a, b, f, eng="dve": op(eng, lambda e: e.tensor_tensor(out=o[:], in0=a[:], in1=b[:], op=f), reads=[a, b], writes=[o])
            def rwkv_s13(j):
                r = RS13[j % 2]
                KR = KRs[j % 2]
                shift(r["qr"], QB[j], j)
                shift(r["qk"], QB[4 + j], 4 + j)
                shift(r["qv"], QB[8 + j], 8 + j)
                op("act", lambda e: e.activation(out=r["qvb"][:], in_=r["qv"][:], func=AF.Copy), reads=[r["qv"]], writes=[r["qvb"]])
                yield
                cs = slice(j * 128, (j + 1) * 128)
                pbw = bank()
                pba = bank()
                op("pe", lambda e, pbw=pbw, cs=cs: e.matmul(pbw[:, 0:NCOL], lhsT=P("wau", rows=slice(0, 64))[:, cs], rhs=TH[0:64, :], start=True, stop=True), reads=[PA, TH], writes=[pbw])
                op("pe", lambda e, pba=pba, cs=cs: e.matmul(pba[:, 0:NCOL], lhsT=P("wau", rows=slice(64, 128))[:, cs], rhs=XWA[64:128, :], start=True, stop=True), reads=[PA, XWA], writes=[pba])
                op("act", lambda e, pbw=pbw, j=j: e.activation(out=r["sgw"][:], in_=pbw[:, 0:NCOL], func=AF.Sigmoid, bias=P("w0", j, j + 1)), reads=[pbw, PA], writes=[r["sgw"]])
                op("act", lambda e, pba=pba, j=j: e.activation(out=r["aa"][:], in_=pba[:, 0:NCOL], func=AF.Sigmoid, bias=P("a0", j, j + 1)), reads=[pba, PA], writes=[r["aa"]])
                pb = bank()
                op("pe", lambda e, pb=pb, cs=cs: e.matmul(pb[:, 0:NCOL], lhsT=P("gup")[:, cs], rhs=SGX[:], start=True, stop=True), reads=[PA, SGX], writes=[pb])
                op("act", lambda e, pb=pb: e.activation(out=r["gg"][:], in_=pb[:, 0:NCOL], func=AF.Copy), reads=[pb], writes=[r["gg"]])
                yield
                if KSTOP <= 1:
                    return
                op("dve", lambda e: e.tensor_scalar(out=r["lw"][:], in0=r["sgw"][:], scalar1=-0.6065306597126334, scalar2=None, op0=ALU.mult), reads=[r["sgw"]], writes=[r["lw"]])
                op("dve", lambda e, j=j: e.tensor_scalar(out=r["kkt"][:], in0=r["qk"][:], scalar1=P("kk", j, j + 1), scalar2=None, op0=ALU.mult), reads=[r["qk"], PA], writes=[r["kkt"]])
                tt2(r["t1"], r["kkt"], r["kkt"], ALU.mult)
                pb = bank()
                op("pe", lambda e, pb=pb: e.matmul(pb[:, 0:NCOL], lhsT=onesblk, rhs=r["t1"][:], start=True, stop=True), reads=[PA, r["t1"]], writes=[pb])
                op("dve", lambda e, pb=pb: e.tensor_scalar(out=r["t2"][:], in0=pb[:, 0:NCOL], scalar1=1e-24, scalar2=None, op0=ALU.max), reads=[pb], writes=[r["t2"]])
                op("act", lambda e: e.activation(out=r["t2"][:], in_=r["t2"][:], func=AF.Sqrt), reads=[r["t2"]], writes=[r["t2"]])
                op("dve", lambda e: e.reciprocal(out=r["t2"][:], in_=r["t2"][:]), reads=[r["t2"]], writes=[r["t2"]])
                tt2(r["kap"], r["kkt"], r["t2"], ALU.mult)
                yield
                op("dve", lambda e, j=j: e.tensor_scalar(out=r["t1"][:], in0=r["aa"][:], scalar1=P("ka", j, j + 1), scalar2=omka[:, j:j + 1], op0=ALU.mult, op1=ALU.add), reads=[r["aa"], PA, omka], writes=[r["t1"]])
                tt2(r["kp"], r["qk"], r["t1"], ALU.mult)
                tt2(r["bet"], r["kap"], r["aa"], ALU.mult)
                op("dve", lambda e, j=j: e.scalar_tensor_tensor(out=r["t1"][:], in0=r["qr"][:], scalar=P("rk", j, j + 1), in1=r["kp"][:], op0=ALU.mult, op1=ALU.mult), reads=[r["qr"], r["kp"], PA], writes=[r["t1"]])
                pb = bank()
                op("pe", lambda e, pb=pb: e.matmul(pb[:, 0:NCOL], lhsT=onesblk, rhs=r["t1"][:], start=True, stop=True), reads=[PA, r["t1"]], writes=[pb])
                op("dve", lambda e, pb=pb: e.tensor_tensor(out=r["bon"][:], in0=pb[:, 0:NCOL], in1=r["qv"][:], op=ALU.mult), reads=[pb, r["qv"]], writes=[r["bon"]])
                yield
                if KSTOP <= 2:
                    return
                op("dve", lambda e: e.tensor_tensor_scan(out=r["cum"][:], data0=P("scanmask"), data1=r["lw"][:], initial=0.0, op0=ALU.mult, op1=ALU.add), reads=[PA, r["lw"]], writes=[r["cum"]])
                op("act", lambda e: e.activation(out=r["Pm"][:], in_=r["cum"][:], func=AF.Exp), reads=[r["cum"]], writes=[r["Pm"]])
                op("act", lambda e: e.activation(out=r["Pinv"][:], in_=r["cum"][:], func=AF.Exp, scale=-1.0), reads=[r["cum"]], writes=[r["Pinv"]])
                tt2(r["t2"], r["cum"], r["lw"], ALU.subtract)
                op("act", lambda e: e.activation(out=r["Ppv"][:], in_=r["t2"][:], func=AF.Exp), reads=[r["t2"]], writes=[r["Ppv"]])
                op("dve", lambda e: e.tensor_tensor(out=KR[:, 0, :], in0=r["kap"][:], in1=r["Ppv"][:], op=ALU.mult), reads=[r["kap"], r["Ppv"]], writes=[KR])
                op("dve", lambda e: e.tensor_tensor(out=KR[:, 1, :], in0=r["qr"][:], in1=r["Pm"][:], op=ALU.mult), reads=[r["qr"], r["Pm"], KR], writes=[KR])
                tt2(r["bt"], r["bet"], r["Pinv"], ALU.mult)
                tt2(r["kt"], r["kp"], r["Pinv"], ALU.mult)
                yield
                if KSTOP <= 3:
                    return
                yield

            def rwkv_s48(j):
                r = RS48[j % 2]
                KR = KRs[j % 2]
                HS = [slice(0, 64), slice(64, 128)]
                f2 = lambda b_, rs: b_[rs, :, :].rearrange("p b c -> p (b c)")
                idb = [identb[HS[h2], 64 * h2:64 * h2 + 64] for h2 in range(2)]
                BCS = [slice(b * 64, (b + 1) * 64) for b in range(NB)]
                pv = [bank(), bank()]
                pk2 = [bank(), bank()]
                for b in range(NB):
                    bc = BCS[b]
                    for h2 in range(2):
                        rs = HS[h2]
                        op("pe", lambda e: e.matmul(pv[h2][rs, b * 64:(b + 1) * 64], lhsT=r["qvb"][rs, bc], rhs=idb[h2], start=True, stop=True), reads=[r["qvb"], identb], writes=[pv[h2]])
                        op("pe", lambda e: e.matmul(pv[h2][rs, 256 + b * 64:256 + (b + 1) * 64], lhsT=r["bt"][rs, bc], rhs=idb[h2], start=True, stop=True), reads=[r["bt"], identb], writes=[pv[h2]])
                        op("pe", lambda e: e.matmul(pk2[h2][rs, b * 64:(b + 1) * 64], lhsT=r["kt"][rs, bc], rhs=idb[h2], start=True, stop=True), reads=[r["kt"], identb], writes=[pk2[h2]])
                for h2 in range(2):
                    rs = HS[h2]
                    op("act", lambda e: e.activation(out=f2(Vt, rs), in_=pv[h2][rs, 0:256], func=AF.Copy), reads=[pv[h2]], writes=[Vt])
                    op("act", lambda e: e.activation(out=f2(Btk, rs), in_=pv[h2][rs, 256:512], func=AF.Copy), reads=[pv[h2]], writes=[Btk])
                    op("act", lambda e: e.activation(out=f2(Ktk, rs), in_=pk2[h2][rs, 0:256], func=AF.Copy), reads=[pk2[h2]], writes=[Ktk])
                yield
                if KSTOP <= 4:
                    return
                pgb = [bank(), bank()]
                pgk = [bank(), bank()]
                pl = [bank(), bank()]
                for b in range(NB):
                    bc = BCS[b]
                    for h2 in range(2):
                        rs = HS[h2]
                        op("pe", lambda e: e.matmul(pl[h2][rs, b * 64:(b + 1) * 64], lhsT=KR[rs, 0, bc], rhs=r["bt"][rs, bc], start=True, stop=True), reads=[r["bt"], KR], writes=[pl[h2]])
                        op("pe", lambda e: e.matmul(pgb[h2][rs, b * 128:(b + 1) * 128], lhsT=r["bt"][rs, bc], rhs=KR[rs, :, bc], start=True, stop=True), reads=[r["bt"], KR], writes=[pgb[h2]])
                        op("pe", lambda e: e.matmul(pgk[h2][rs, b * 128:(b + 1) * 128], lhsT=r["kt"][rs, bc], rhs=KR[rs, :, bc], start=True, stop=True), reads=[r["kt"], KR], writes=[pgk[h2]])
                for h2 in range(2):
                    rs = HS[h2]
                    mG = P("maskG", rows=rs).unsqueeze(1).to_broadcast([64, NB, 128])
                    mL = P("maskL", rows=rs).unsqueeze(1).to_broadcast([64, NB, 64])
                    i64 = P("ident", 64 * h2, 64 * h2 + 64, rows=rs).unsqueeze(1).to_broadcast([64, NB, 64])
                    op("dve", lambda e: e.tensor_tensor(out=Xa[0][rs, :, :], in0=pl[h2][rs, 0:256].rearrange("p (u c) -> p u c", u=NB), in1=mL, op=ALU.mult), reads=[pl[h2], PA], writes=[Xa[0]])
                    op("dve", lambda e: e.tensor_tensor(out=GbS[rs, :, :], in0=pgb[h2][rs, :].rearrange("p (u c) -> p u c", u=NB), in1=mG, op=ALU.mult), reads=[pgb[h2], PA], writes=[GbS])
                    op("dve", lambda e: e.tensor_tensor(out=Wt[rs, :, :], in0=i64, in1=GbS[rs, :, 0:64], op=ALU.subtract), reads=[PA, GbS], writes=[Wt])
                    op("act", lambda e: e.activation(out=XTa[0][rs, :, :], in_=GbS[rs, :, 0:64], func=AF.Copy), reads=[GbS], writes=[XTa[0]])
                    op("act", lambda e: e.activation(out=Wtb[rs, :, :], in_=Wt[rs, :, :], func=AF.Copy), reads=[Wt], writes=[Wtb])
                    op("dve", lambda e: e.tensor_tensor(out=GkS[rs, :, :], in0=pgk[h2][rs, :].rearrange("p (u c) -> p u c", u=NB), in1=mG, op=ALU.mult), reads=[pgk[h2], PA], writes=[GkS])
                yield
                if KSTOP <= 5:
                    return
                for lvl in range(1, 6):
                    Xp = Xa[(lvl - 1) % 2]
                    Xn = Xa[lvl % 2]
                    XTn = XTa[lvl % 2]
                    XTp_buf, XTp_ap = XTa[(lvl - 1) % 2], (lambda rs, b, t_=XTa[(lvl - 1) % 2]: t_[rs, b, :])
                    px = [bank(), bank()]
                    for b in range(NB):
                        for h2 in range(2):
                            rs = HS[h2]
                            op("pe", lambda e: e.matmul(px[h2][rs, b * 64:(b + 1) * 64], lhsT=XTp_ap(rs, b), rhs=Xp[rs, b, :], start=True, stop=True), reads=[Xp, XTp_buf], writes=[px[h2]])
                    for h2 in range(2):
                        rs = HS[h2]
                        op("act", lambda e: e.activation(out=f2(Xn, rs), in_=px[h2][rs, 0:256], func=AF.Copy), reads=[px[h2]], writes=[Xn])
                    if lvl < 5:
                        pxt = [bank(), bank()]
                        for b in range(NB):
                            for h2 in range(2):
                                rs = HS[h2]
                                op("pe", lambda e: e.matmul(pxt[h2][rs, b * 64:(b + 1) * 64], lhsT=Xp[rs, b, :], rhs=XTp_ap(rs, b), start=True, stop=True), reads=[Xp, XTp_buf], writes=[pxt[h2]])
                        for h2 in range(2):
                            rs = HS[h2]
                            op("act", lambda e: e.activation(out=f2(XTn, rs), in_=pxt[h2][rs, 0:256], func=AF.Copy), reads=[pxt[h2]], writes=[XTn])
                    pw = [bank(), bank()]
                    for b in range(NB):
                        for h2 in range(2):
                            rs = HS[h2]
                            op("pe", lambda e: e.matmul(pw[h2][rs, b * 64:(b + 1) * 64], lhsT=Xn[rs, b, :], rhs=Wtb[rs, b, :], start=True, stop=True), reads=[Xn, Wtb], writes=[pw[h2]])
                    for h2 in range(2):
                        rs = HS[h2]
                        op("dve", lambda e: e.tensor_tensor(out=f2(Wt, rs), in0=pw[h2][rs, 0:256], in1=f2(Wt, rs), op=ALU.add), reads=[pw[h2], Wt], writes=[Wt])
                        op("act", lambda e: e.activation(out=f2(Wtb, rs), in_=f2(Wt, rs), func=AF.Copy), reads=[Wt], writes=[Wtb])
                    yield
                yield
                if KSTOP <= 6:
                    return
                STj = ST[j]
                pr = [bank(), bank()]
                for b in range(NB):
                    bc = BCS[b]
                    for h2 in range(2):
                        rs = HS[h2]
                        op("pe", lambda e: e.matmul(pr[h2][rs, b * 64:(b + 1) * 64], lhsT=KR[rs, 0, bc], rhs=STb[j][rs, b, :], start=True, stop=False), reads=[KR, STb[j]], writes=[pr[h2]])
                        op("pe", lambda e: e.matmul(pr[h2][rs, b * 64:(b + 1) * 64], lhsT=GkS[rs, b, 0:64], rhs=Vt[rs, b, :], start=False, stop=True), reads=[GkS, Vt], writes=[pr[h2]])
                for h2 in range(2):
                    rs = HS[h2]
                    op("act", lambda e: e.activation(out=f2(Rn, rs), in_=pr[h2][rs, 0:256], func=AF.Copy, scale=-1.0), reads=[pr[h2]], writes=[Rn])
                pu = [bank(), bank()]
                for b in range(NB):
                    for h2 in range(2):
                        rs = HS[h2]
                        op("pe", lambda e: e.matmul(pu[h2][rs, b * 64:(b + 1) * 64], lhsT=Wtb[rs, b, :], rhs=Rn[rs, b, :], start=True, stop=True), reads=[Wtb, Rn], writes=[pu[h2]])
                for h2 in range(2):
                    rs = HS[h2]
                    op("act", lambda e: e.activation(out=f2(Uu, rs), in_=pu[h2][rs, 0:256], func=AF.Copy), reads=[pu[h2]], writes=[Uu])
                yield
                if KSTOP <= 7:
                    return
                po = [bank(), bank()]
                psn = [bank(), bank()]
                for b in range(NB):
                    bc = BCS[b]
                    for h2 in range(2):
                        rs = HS[h2]
                        op("pe", lambda e: e.matmul(po[h2][rs, bc], lhsT=STb[j][rs, b, :], rhs=KR[rs, 1, bc], start=True, stop=False), reads=[STb[j], KR], writes=[po[h2]])
                        op("pe", lambda e: e.matmul(po[h2][rs, bc], lhsT=Uu[rs, b, :], rhs=GbS[rs, b, 64:128], start=False, stop=False), reads=[Uu, GbS], writes=[po[h2]])
                        op("pe", lambda e: e.matmul(po[h2][rs, bc], lhsT=Vt[rs, b, :], rhs=GkS[rs, b, 64:128], start=False, stop=True), reads=[Vt, GkS], writes=[po[h2]])
                        op("pe", lambda e: e.matmul(psn[h2][rs, bc], lhsT=Btk[rs, b, :], rhs=Uu[rs, b, :], start=True, stop=False), reads=[Btk, Uu], writes=[psn[h2]])
                        op("pe", lambda e: e.matmul(psn[h2][rs, bc], lhsT=Ktk[rs, b, :], rhs=Vt[rs, b, :], start=False, stop=True), reads=[Ktk, Vt], writes=[psn[h2]])
                for h2 in range(2):
                    rs = HS[h2]
                    op("act", lambda e: e.activation(out=r["oT"][rs, :], in_=po[h2][rs, 0:NCOL], func=AF.Copy), reads=[po[h2]], writes=[r["oT"]])
                    op("dve", lambda e: e.tensor_tensor(out=r["t1"][rs, :], in0=psn[h2][rs, 0:NCOL], in1=f2(STj, rs), op=ALU.add), reads=[psn[h2], STj], writes=[r["t1"]])
                    pT = r["Pm"][rs, :].rearrange("p (b t) -> p b t", b=NB)[:, :, TC - 1:TC].to_broadcast([64, NB, 64])
                    op("dve", lambda e: e.tensor_tensor(out=STj[rs, :, :], in0=r["t1"][rs, :].rearrange("p (b c) -> p b c", b=NB), in1=pT, op=ALU.mult), reads=[r["t1"], r["Pm"]], writes=[STj])
                    op("act", lambda e: e.activation(out=STb[j][rs, :, :], in_=STj[rs, :, :], func=AF.Copy), reads=[STj], writes=[STb[j]])
                yield
                if KSTOP <= 8:
                    return
                pb = bank()
                op("pe", lambda e, pb=pb: e.matmul(pb[:, 0:NCOL], lhsT=onesblk, rhs=r["oT"][:], start=True, stop=True), reads=[PA, r["oT"]], writes=[pb])
                op("dve", lambda e, pb=pb: e.scalar_tensor_tensor(out=r["cen"][:], in0=pb[:, 0:NCOL], scalar=-1.0 / 64, in1=r["oT"][:], op0=ALU.mult, op1=ALU.add), reads=[pb, r["oT"]], writes=[r["cen"]])
                tt2(r["t1"], r["cen"], r["cen"], ALU.mult)
                pb = bank()
                op("pe", lambda e, pb=pb: e.matmul(pb[:, 0:NCOL], lhsT=onesblk, rhs=r["t1"][:], start=True, stop=True), reads=[PA, r["t1"]], writes=[pb])
                op("dve", lambda e, pb=pb: e.tensor_scalar(out=r["t2"][:], in0=pb[:, 0:NCOL], scalar1=1.0 / 64, scalar2=GN_EPS, op0=ALU.mult, op1=ALU.add), reads=[pb], writes=[r["t2"]])
                op("act", lambda e: e.activation(out=r["t2"][:], in_=r["t2"][:], func=AF.Sqrt), reads=[r["t2"]], writes=[r["t2"]])
                op("dve", lambda e: e.reciprocal(out=r["t2"][:], in_=r["t2"][:]), reads=[r["t2"]], writes=[r["t2"]])
                tt2(r["cen"], r["cen"], r["t2"], ALU.mult)
                op("dve", lambda e, j=j: e.tensor_scalar(out=r["cen"][:], in0=r["cen"][:], scalar1=P("gnw", j, j + 1), scalar2=P("gnb", j, j + 1), op0=ALU.mult, op1=ALU.add), reads=[r["cen"], PA], writes=[r["cen"]])
                tt2(r["cen"], r["cen"], r["bon"], ALU.add)
                op("dve", lambda e, j=j: e.tensor_tensor(out=mixin[:, 6 + j, :, :].rearrange("p b t -> p (b t)"), in0=r["cen"][:], in1=r["gg"][:], op=ALU.mult), reads=[r["cen"], r["gg"]], writes=[mixin])

            n_s5 = 0 if "s" in SKIP else TC // SUB
            n_rw = 0 if "r" in SKIP else 4

            def run_all(g):
                for _ in g:
                    pass

            def zipgen(ga, gb):
                alive = [ga, gb]
                while alive:
                    for g in list(alive):
                        try:
                            next(g)
                        except StopIteration:
                            alive.remove(g)

            if n_rw:
                run_all(rwkv_s13(0))
            for i_ in range(4):
                if i_ < n_s5:
                    s5_front(i_)
                if i_ < n_rw:
                    if i_ + 1 < n_rw:
                        zipgen(rwkv_s48(i_), rwkv_s13(i_ + 1))
                    else:
                        run_all(rwkv_s48(i_))
                if i_ < n_s5:
                    s5_back(i_)
            tap("mixs5", mixin, mixin[:, 0:6, :, :])

            for ct in range(8):
                pb = bank()
                for k in range(10):
                    kr = TR[k] if k < 6 else 128
                    op("pe", lambda e, pb=pb, k=k, ct=ct, kr=kr: e.matmul(pb[:, 0:NCOL], lhsT=Wout[0:kr, k, ct * 128:(ct + 1) * 128], rhs=mixin[0:kr, k, :, :], start=(k == 0), stop=(k == 9)), reads=[Wout, mixin], writes=[pb])
                op("act", lambda e, pb=pb, ct=ct: e.activation(out=mixT[:, ct, :], in_=pb[:, 0:NCOL], func=AF.Copy), reads=[pb], writes=[mixT])
            for tt in range(2):
                for kh in range(2):
                    pb = bank()
                    for k4 in range(4):
                        k = kh * 4 + k4
                        op("pe", lambda e, pb=pb, k=k, k4=k4, tt=tt: e.transpose(pb[:, k4 * 128:(k4 + 1) * 128], mixT[:, k, tt * 128:(tt + 1) * 128], ident), reads=[mixT, PA], writes=[pb])
                    op("dve", lambda e, pb=pb, kh=kh: e.tensor_copy(out=tokB[:, kh * 512:(kh + 1) * 512], in_=pb[:, :]), reads=[pb], writes=[tokB])
                op("dve", lambda e, tt=tt: e.scalar_tensor_tensor(out=tokA[:], in0=tokB[:], scalar=1.0, in1=tokB[:], op0=ALU.mult, op1=ALU.mult, accum_out=sst[:, 2 + tt:3 + tt]), reads=[tokB], writes=[tokA, sst])
                rstd_from_ss(2 + tt)
                op("dve", lambda e, tt=tt: e.scalar_tensor_tensor(out=tokA[:], in0=tokB[:], scalar=sst[:, 2 + tt:3 + tt], in1=P("gBmix"), op0=ALU.mult, op1=ALU.mult), reads=[tokB, sst, PA], writes=[tokA])
                op("dve", lambda e, tt=tt: e.tensor_tensor(out=tokA[:], in0=tokA[:], in1=xtok[tt][:], op=ALU.add), reads=[tokA, xtok[tt]], writes=[tokA])
                for b2 in range(2):
                    dma("sp", None, hsc[2 * tt + b2, t0:t0 + TC, :], tokA[64 * b2:64 * b2 + 64, :], reads=[tokA], out_final=True)
        fw.emit()
    if not do_ffn:
        fw.stack.close()
        return nc

    fw2 = fw
    fw2.final_tokens = []
    with fw2.tscope():
        sb, op, dma = fw2.sb, fw2.op, fw2.dma
        PB = sb("PB", [128, nB], F32)

        def P2(name, c0=0, c1=None):
            o, n = offB[name]
            c1 = n if c1 is None else c1
            return PB[:, o + c0:o + c1]
        Wup = sb("Wup", [128, 8, DFF], BF16)
        Wdn = sb("Wdn", [128, 32, D], BF16)
        banks = [fw2.ps(f"bankb{i}", [128, 512], F32) for i in range(8)]
        bi = [0]

        def bank():
            b = banks[bi[0] % 8]
            bi[0] += 1
            return b
        dma("sp", PB, PB[:], ppb)
        for k in range(8):
            for h in range(2):
                dma("pool", Wup, Wup[:, k, h * 2048:(h + 1) * 2048], w_up[k * 128:(k + 1) * 128, h * 2048:(h + 1) * 2048])
        for k in range(32):
            dma("pool", Wdn, Wdn[:, k, :], w_dn[k * 128:(k + 1) * 128, :])
        htok = [sb(f"htok{i}", [128, D], F32) for i in range(2)]
        tokA = sb("tokA2", [128, D], F32)
        tokB = sb("tokB2", [128, D], F32)
        sst = sb("sst2", [128, 8], F32)
        hnT = sb("hnT", [128, 8, NCOL], BF16)
        hid = sb("hid", [128, 32, NCOL], BF16)
        rl = [sb(f"rl{i}", [128, NCOL], F32) for i in range(2)]
        ffT = sb("ffT", [128, 8, NCOL], F32)
        ident = P2("ident")

        def rstd_from_ss2(col):
            op("dve", lambda e: e.tensor_scalar(out=sst[:, col:col + 1], in0=sst[:, col:col + 1], scalar1=1.0 / D, scalar2=1e-6, op0=ALU.mult, op1=ALU.add), reads=[sst], writes=[sst])
            op("act", lambda e: e.activation(out=sst[:, col:col + 1], in_=sst[:, col:col + 1], func=AF.Sqrt), reads=[sst], writes=[sst])
            op("dve", lambda e: e.reciprocal(out=sst[:, col:col + 1], in_=sst[:, col:col + 1]), reads=[sst], writes=[sst])

        for it in range(nit):
            t0 = it * TC
            for tt in range(2):
                for b2 in range(2):
                    dma("sp", htok[tt], htok[tt][64 * b2:64 * b2 + 64, :], hsc[2 * tt + b2, t0:t0 + TC, :])
            for tt in range(2):
                op("dve", lambda e, tt=tt: e.scalar_tensor_tensor(out=tokA[:], in0=htok[tt][:], scalar=1.0, in1=htok[tt][:], op0=ALU.mult, op1=ALU.mult, accum_out=sst[:, tt:tt + 1]), reads=[htok[tt]], writes=[tokA, sst])
                rstd_from_ss2(tt)
                op("dve", lambda e, tt=tt: e.tensor_scalar(out=tokA[:], in0=htok[tt][:], scalar1=sst[:, tt:tt + 1], scalar2=None, op0=ALU.mult), reads=[htok[tt], sst], writes=[tokA])
                for kh in range(2):
                    pb = bank()
                    for k4 in range(4):
                        k = kh * 4 + k4
                        op("pe", lambda e, pb=pb, k=k, k4=k4: e.transpose(pb[:, k4 * 128:(k4 + 1) * 128], tokA[:, k * 128:(k + 1) * 128], ident), reads=[tokA, PB], writes=[pb])
                    gpre = P2("gpremlp", kh * 4, kh * 4 + 4).unsqueeze(2).to_broadcast([128, 4, 128])
                    op("dve", lambda e, pb=pb, kh=kh, tt=tt, gpre=gpre: e.tensor_tensor(out=hnT[:, kh * 4:kh * 4 + 4, tt * 128:(tt + 1) * 128], in0=pb[:, :].rearrange("p (k c) -> p k c", k=4), in1=gpre, op=ALU.mult), reads=[pb, PB], writes=[hnT])
            for ht in range(32):
                pb = bank()
                for k in range(8):
                    op("pe", lambda e, pb=pb, k=k, ht=ht: e.matmul(pb[:, 0:NCOL], lhsT=Wup[:, k, ht * 128:(ht + 1) * 128], rhs=hnT[:, k, :], start=(k == 0), stop=(k == 7)), reads=[Wup, hnT], writes=[pb])
                rr = rl[ht % 2]
                op("act", lambda e, pb=pb, rr=rr: e.activation(out=rr[:], in_=pb[:, 0:NCOL], func=AF.Relu), reads=[pb], writes=[rr])
                op("pool", lambda e, rr=rr, ht=ht: e.tensor_tensor(out=hid[:, ht, :], in0=rr[:], in1=rr[:], op=ALU.mult), reads=[rr], writes=[hid])
            for ct in range(8):
                pb = bank()
                for k in range(32):
                    op("pe", lambda e, pb=pb, k=k, ct=ct: e.matmul(pb[:, 0:NCOL], lhsT=Wdn[:, k, ct * 128:(ct + 1) * 128], rhs=hid[:, k, :], start=(k == 0), stop=(k == 31)), reads=[Wdn, hid], writes=[pb])
                op("act", lambda e, pb=pb, ct=ct: e.activation(out=ffT[:, ct, :], in_=pb[:, 0:NCOL], func=AF.Copy), reads=[pb], writes=[ffT])
            for tt in range(2):
                for kh in range(2):
                    pb = bank()
                    for k4 in range(4):
                        k = kh * 4 + k4
                        op("pe", lambda e, pb=pb, k=k, k4=k4, tt=tt: e.transpose(pb[:, k4 * 128:(k4 + 1) * 128], ffT[:, k, tt * 128:(tt + 1) * 128], ident), reads=[ffT, PB], writes=[pb])
                    op("dve", lambda e, pb=pb, kh=kh: e.tensor_copy(out=tokB[:, kh * 512:(kh + 1) * 512], in_=pb[:, :]), reads=[pb], writes=[tokB])
                op("dve", lambda e, tt=tt: e.scalar_tensor_tensor(out=tokA[:], in0=tokB[:], scalar=1.0, in1=tokB[:], op0=ALU.mult, op1=ALU.mult, accum_out=sst[:, 2 + tt:3 + tt]), reads=[tokB], writes=[tokA, sst])
                rstd_from_ss2(2 + tt)
                op("dve", lambda e, tt=tt: e.scalar_tensor_tensor(out=tokA[:], in0=tokB[:], scalar=sst[:, 2 + tt:3 + tt], in1=P2("gBmlp"), op0=ALU.mult, op1=ALU.mult), reads=[tokB, sst, PB], writes=[tokA])
                op("dve", lambda e, tt=tt: e.tensor_tensor(out=tokA[:], in0=tokA[:], in1=htok[tt][:], op=ALU.add), reads=[tokA, htok[tt]], writes=[tokA])
                for b2 in range(2):
                    dma("sp", None, out[2 * tt + b2, t0:t0 + TC, :], tokA[64 * b2:64 * b2 + 64, :], reads=[tokA], out_final=True)
        fw2.emit()
    fw.stack.close()
    return nc


_CACHE = {}


def kernel(**inputs):
    inp = {k: np.asarray(v) for k, v in inputs.items()}
    A, B = build_params(inp)
    ppa, ppb = A.pack(), B.pack()
    key = "full"
    if key not in _CACHE:
        _CACHE[key] = build_program(A.off, A.n, B.off, B.n)
    nc = _CACHE[key]
    x = np.ascontiguousarray(inp["x"], dtype=np.float32)
    sq = lambda k: np.ascontiguousarray(inp[k][0], dtype=np.float32)
    shared = {"ppa": ppa, "ppb": ppb, "w_in": sq("w_in"), "w_out": sq("w_out"), "w_glu": sq("s5_w_glu"),
              "w_ff_up": sq("w_ff_up"), "w_ff_down": sq("w_ff_down")}
    in_maps = [dict(shared, x=np.ascontiguousarray(x[c * NB:(c + 1) * NB])) for c in range(NCORES)]
    res = run_bass_kernel_spmd(nc, in_maps, core_ids=list(range(NCORES)))
    return np.concatenate([r["out"] for r in res.results], axis=0)
```

```python
import contextlib
import types
import numpy as np
import concourse.bass as bass
import concourse.mybir as mybir
from concourse.bass_utils import run_bass_kernel_spmd

F32 = mybir.dt.float32
BF16 = mybir.dt.bfloat16
AF = mybir.ActivationFunctionType
ALU = mybir.AluOpType


def _freeze(fn):
    if fn.__closure__ is None:
        return fn
    cells = []
    for c in fn.__closure__:
        try:
            cells.append(types.CellType(c.cell_contents))
        except ValueError:
            cells.append(c)
    g = types.FunctionType(fn.__code__, fn.__globals__, fn.__name__, fn.__defaults__, tuple(cells))
    g.__kwdefaults__ = fn.__kwdefaults__
    return g


class Buf:
    def __init__(self, fw, name, t):
        self.fw = fw
        self.name = name
        self.t = t
        self.last_w = None
        self.readers = []
        self.ld_sem = None
        self.ld_cnt = 0
        self.st_sem = None
        self.st_cnt = 0

    def __getitem__(self, k):
        return self.t[k]


class FW:
    ENG = ("pe", "act", "dve", "pool", "sp")

    def __init__(self, nc):
        self.nc = nc
        self.stack = contextlib.ExitStack()
        self.tstack = contextlib.ExitStack()
        self.eng_obj = {"pe": nc.tensor, "act": nc.scalar, "dve": nc.vector,
                        "pool": nc.gpsimd, "sp": nc.sync}
        self.sem = {}
        self.cnt = {e: 0 for e in self.ENG}
        self.prog = {e: [] for e in self.ENG}
        self.waited = {e: {} for e in self.ENG}
        self.final_tokens = []
        self.all_sems = []
        self.same_engine_sync = True
        self.nsem = 0

    def scope(self):
        return self.stack

    def tscope(self):
        self.tstack = contextlib.ExitStack()
        return self.tstack

    def new_sem(self, name):
        self.nsem += 1
        s = self.stack.enter_context(self.nc.semaphore(f"{name}_{self.nsem}"))
        self.all_sems.append(s)
        return s

    def init_sems(self):
        for e in self.ENG:
            if e not in self.sem:
                self.sem[e] = self.new_sem("s_" + e)

    NOZERO = ("PA", "PB", "Win", "Wup", "Wdn")

    def sb(self, name, shape, dtype):
        t = self.tstack.enter_context(self.nc.sbuf_tensor(name, list(shape), dtype))
        b = Buf(self, name, t)
        if name not in self.NOZERO:
            self.op("pool", lambda e: e.memset(b[:], 0.0), writes=[b])
        return b

    def ps(self, name, shape, dtype):
        t = self.tstack.enter_context(self.nc.psum_tensor(name, list(shape), dtype))
        b = Buf(self, name, t)
        self.op("dve", lambda e: e.memset(b[:], 0.0), writes=[b])
        return b

    def view(self, name, ap):
        return Buf(self, name, ap)

    def _deps(self, reads, writes):
        deps = []
        for b in reads:
            if b.last_w is not None:
                deps.append(b.last_w)
        for b in writes:
            if b.last_w is not None:
                deps.append(b.last_w)
            deps.extend(b.readers)
        return deps

    def _emit_waits(self, eng, deps):
        w = self.waited[eng]
        for (sem, val, src) in deps:
            if src == eng and (eng == "pe" or not self.same_engine_sync):
                continue
            key = id(sem)
            if w.get(key, 0) >= val:
                continue
            w[key] = val
            self.prog[eng].append(("wait", sem, val))

    def op(self, eng, fn, reads=(), writes=()):
        self.init_sems()
        reads = [b for b in reads if b is not None]
        writes = [b for b in writes if b is not None]
        self._emit_waits(eng, self._deps(reads, writes))
        self.cnt[eng] += 1
        tok = (self.sem[eng], self.cnt[eng], eng)
        self.prog[eng].append(("op", _freeze(fn), self.sem[eng], 1))
        for b in reads:
            b.readers.append(tok)
        for b in writes:
            b.last_w = tok
            b.readers = []
        return tok

    def dma(self, q, dst, out_ap, in_ap, reads=(), out_final=False, **kw):
        self.init_sems()
        reads = [b for b in reads if b is not None]
        writes = [dst] if dst is not None else []
        self._emit_waits(q, self._deps(reads, writes))
        if dst is not None:
            if dst.ld_sem is None:
                dst.ld_sem = self.new_sem("ld_" + dst.name)
            dst.ld_cnt += 16
            tok = (dst.ld_sem, dst.ld_cnt, "dma")
            sem = dst.ld_sem
            dst.last_w = tok
            dst.readers = []
            for b in reads:
                b.readers.append(tok)
        else:
            src = reads[0]
            if src.st_sem is None:
                src.st_sem = self.new_sem("st_" + src.name)
            src.st_cnt += 16
            tok = (src.st_sem, src.st_cnt, "dma")
            sem = src.st_sem
            for b in reads:
                b.readers.append(tok)
            if out_final:
                self.final_tokens.append(tok)
        fn = (lambda e, o=out_ap, i=in_ap, k=kw: e.dma_start(out=o, in_=i, **k))
        self.prog[q].append(("op", fn, sem, 16))
        return tok

    def drain_all(self):
        self.init_sems()
        deps = [(self.sem[e], self.cnt[e], e) for e in self.ENG if e != "sp" and self.cnt[e] > 0]
        deps += self.final_tokens
        self._emit_waits("sp", deps)

    def emit(self):
        self.drain_all()
        nc = self.nc
        with nc.Block() as block:
            def mk(eng):
                def body(e):
                    for item in self.prog[eng]:
                        if item[0] == "wait":
                            e.wait_ge(item[1], item[2])
                        else:
                            ins = item[1](e)
                            ins.then_inc(item[2], item[3])
                return body
            block.tensor(mk("pe"))
            block.scalar(mk("act"))
            block.vector(mk("dve"))
            block.gpsimd(mk("pool"))
            block.sync(mk("sp"))
        self.prog = {e: [] for e in self.ENG}


NCORES = 8
NB = 4
SEQ = 2048
D = 1024
TC = 64
NIT = SEQ // TC
NCOL = NB * TC
DS5 = 512
NPROJ = 2304
DFF = 4096
SUB = 16
GN_EPS = 64e-5


class PP:
    def __init__(self):
        self.off = {}
        self.n = 0
        self.parts = []

    def add(self, name, arr):
        arr = np.ascontiguousarray(arr, dtype=np.float32)
        if arr.shape[0] < 128:
            pad = np.zeros((128 - arr.shape[0],) + arr.shape[1:], np.float32)
            arr = np.concatenate([arr, pad], 0)
        arr = arr.reshape(128, -1)
        self.off[name] = (self.n, arr.shape[1])
        self.n += arr.shape[1]
        self.parts.append(arr)

    def pack(self):
        return np.ascontiguousarray(np.concatenate(self.parts, 1))


def pk(v, nt):
    return np.asarray(v, np.float32).reshape(nt, 128).T


def build_params(inp):
    g = lambda k: np.asarray(inp[k], np.float32)[0]
    A = PP()
    A.add("gpre", pk(g("g_pre_mix"), 8))
    s5l = lambda a: a.reshape(16, 2, 64).transpose(1, 2, 0).reshape(128, 16)
    A.add("lamre", s5l(g("s5_lam_re")))
    A.add("lamim", s5l(g("s5_lam_im")))
    A.add("logdt", s5l(np.repeat(g("s5_log_dt")[:, None], 64, 1)))
    s5b = lambda a: a.reshape(16, 2, 64, 16).transpose(1, 2, 0, 3).reshape(128, 16 * 16)
    A.add("bre", s5b(g("s5_b_re")))
    A.add("bim", s5b(g("s5_b_im")))

    def ctl(c):
        o = np.zeros((2, 64, 16, 2, 16), np.float32)
        cc = c.reshape(16, 2, 16, 64)
        for g2 in range(2):
            o[g2, :, :, g2, :] = cc[:, g2].transpose(2, 0, 1)
        return o.reshape(128, 16 * 32)
    A.add("ctre", ctl(g("s5_c_re")))
    A.add("ctim", ctl(g("s5_c_im")))
    def pk96(v):
        o = np.zeros((128, 6), np.float32)
        for tl in range(6):
            n = 96 if tl < 5 else 32
            o[:n, tl] = v[96 * tl:96 * tl + n]
        return o
    A.add("dskip", pk96(g("s5_d")))
    A.add("bglu", pk96(g("s5_b_glu")))
    A.add("mu", pk(g("rw_mu"), 14))
    A.add("w0", pk(g("rw_w0"), 4))
    A.add("a0", pk(g("rw_a0"), 4))
    A.add("kk", pk(g("rw_k_k"), 4))
    A.add("ka", pk(g("rw_k_a"), 4))
    A.add("rk", pk(g("rw_r_k").reshape(-1), 4))
    A.add("gnw", pk(g("rw_gn_w"), 4))
    A.add("gnb", pk(g("rw_gn_b"), 4))
    A.add("wau", np.concatenate([g("rw_w_up"), g("rw_a_up")], 0))
    A.add("gup", g("rw_g_up"))
    A.add("gBmix", np.repeat(g("g_post_mix")[None, :], 128, 0))
    A.add("ident", np.eye(128, dtype=np.float32))
    ob = np.zeros((128, 128), np.float32)
    ob[:64, :64] = 1
    ob[64:, 64:] = 1
    A.add("onesblk", ob)
    j = np.arange(64)[:, None]
    t = np.arange(64)[None, :]
    mg = np.concatenate([(t > j), (t >= j)], 1).astype(np.float32)
    A.add("maskG", np.concatenate([mg, mg], 0))
    A.add("maskL", np.concatenate([(j > t), (j > t)], 0).astype(np.float32))
    sm = np.ones((128, NB, TC), np.float32)
    sm[:, :, 0] = 0
    A.add("scanmask", sm.reshape(128, NCOL))
    B = PP()
    B.add("gpremlp", pk(g("g_pre_mlp"), 8))
    B.add("gBmlp", np.repeat(g("g_post_mlp")[None, :], 128, 0))
    B.add("ident", np.eye(128, dtype=np.float32))
    return A, B


def build_program(offA, nA, offB, nB, nit=NIT, do_ffn=True):
    nc = bass.Bass("TRN2", target_bir_lowering=False)
    x = nc.dram_tensor("x", [NB, SEQ, D], F32, kind="ExternalInput").ap()
    ppa = nc.dram_tensor("ppa", [128, nA], F32, kind="ExternalInput").ap()
    ppb = nc.dram_tensor("ppb", [128, nB], F32, kind="ExternalInput").ap()
    w_in = nc.dram_tensor("w_in", [D, NPROJ], F32, kind="ExternalInput").ap()
    w_out = nc.dram_tensor("w_out", [D, D], F32, kind="ExternalInput").ap()
    w_glu = nc.dram_tensor("w_glu", [DS5, DS5], F32, kind="ExternalInput").ap()
    w_up = nc.dram_tensor("w_ff_up", [D, DFF], F32, kind="ExternalInput").ap()
    w_dn = nc.dram_tensor("w_ff_down", [DFF, D], F32, kind="ExternalInput").ap()
    out = nc.dram_tensor("out", [NB, SEQ, D], F32, kind="ExternalOutput").ap()
    hsc = nc.dram_tensor("hsc", [NB, SEQ, D], F32, kind="ExternalOutput").ap()

    import os
    SKIP = os.environ.get("KSKIP", "")
    TAPS = set(os.environ.get("KTAPS", "").split(",")) - {""}
    KSTOP = int(os.environ.get("KSTOP", "99"))
    SCAN_ENG = os.environ.get("KSCAN", "pool")
    fw = FW(nc)
    tapped = set()

    def tap(name, buf, ap):
        if name not in TAPS or name in tapped:
            return
        tapped.add(name)
        dt_ = ap.dtype
        dd = nc.dram_tensor("dbg_" + name, list(ap.shape), dt_, kind="ExternalOutput").ap()
        fw.dma("sp", None, dd, ap, reads=[buf], out_final=True)
    fw.stack.__enter__()
    with fw.tscope():
        sb, op, dma = fw.sb, fw.op, fw.dma
        PA = sb("PA", [128, nA], F32)

        def P(name, c0=0, c1=None, rows=slice(0, 128)):
            o, n = offA[name]
            c1 = n if c1 is None else c1
            return PA[rows, o + c0:o + c1]
        Win = sb("Win", [128, 8, NPROJ], BF16)
        Wout = sb("Wout", [128, 10, D], BF16)
        Wglu = sb("Wglu", [128, 6, DS5], BF16)
        TR = [96, 96, 96, 96, 96, 32]
        banks = [fw.ps(f"bank{i}", [128, 512], F32) for i in range(8)]
        bi = [0]

        def bank():
            b = banks[bi[0] % 8]
            bi[0] += 1
            return b

        dma("sp", PA, PA[:], ppa)
        for k in range(8):
            for h in range(2):
                dma("pool", Win, Win[:, k, h * 1152:(h + 1) * 1152], w_in[k * 128:(k + 1) * 128, h * 1152:(h + 1) * 1152])
        for k in range(6):
            dma("pool", Wout, Wout[0:TR[k], k, :], w_out[k * 96:k * 96 + TR[k], :])
            dma("pool", Wglu, Wglu[0:TR[k], k, :], w_glu[k * 96:k * 96 + TR[k], :])
        for k in range(4):
            dma("pool", Wout, Wout[:, 6 + k, :], w_out[512 + k * 128:512 + (k + 1) * 128, :])

        s5t = sb("s5t", [128, 12, 16], F32)
        T = lambda i: s5t[:, i, :]
        AR, AI, FR, FI = 0, 1, 2, 3
        tt_ = lambda o, a, b, f: op("dve", lambda e: e.tensor_tensor(out=o, in0=a, in1=b, op=f), reads=[PA, s5t], writes=[s5t])
        ts_ = lambda o, a, s1, s2, f1, f2: op("dve", lambda e: e.tensor_scalar(out=o, in0=a, scalar1=s1, scalar2=s2, op0=f1, op1=f2), reads=[PA, s5t], writes=[s5t])
        ac_ = lambda o, a, f, **kw: op("act", lambda e: e.activation(out=o, in_=a, func=f, **kw), reads=[PA, s5t], writes=[s5t])
        ac_(T(4), P("logdt"), AF.Exp)
        tt_(T(5), P("lamre"), T(4), ALU.mult)
        tt_(T(6), P("lamim"), T(4), ALU.mult)
        ac_(T(7), T(5), AF.Exp)
        ts_(T(8), T(6), 1.0 / 16, None, ALU.mult, ALU.bypass)
        ts_(T(9), T(6), 1.0 / 16, float(np.pi / 2), ALU.mult, ALU.add)
        ac_(T(8), T(8), AF.Sin)
        ac_(T(9), T(9), AF.Sin)
        for _ in range(4):
            tt_(T(10), T(8), T(9), ALU.mult)
            tt_(T(11), T(8), T(8), ALU.mult)
            tt_(T(9), T(9), T(9), ALU.mult)
            tt_(T(9), T(9), T(11), ALU.subtract)
            ts_(T(8), T(10), 2.0, None, ALU.mult, ALU.bypass)
        tt_(T(AR), T(7), T(9), ALU.mult)
        tt_(T(AI), T(7), T(8), ALU.mult)
        tt_(T(4), P("lamre"), P("lamre"), ALU.mult)
        tt_(T(5), P("lamim"), P("lamim"), ALU.mult)
        tt_(T(4), T(4), T(5), ALU.add)
        op("dve", lambda e: e.reciprocal(out=T(4), in_=T(4)), reads=[s5t], writes=[s5t])
        ts_(T(5), T(AR), -1.0, None, ALU.add, ALU.bypass)
        tt_(T(6), T(5), P("lamre"), ALU.mult)
        tt_(T(7), T(AI), P("lamim"), ALU.mult)
        tt_(T(6), T(6), T(7), ALU.add)
        tt_(T(FR), T(6), T(4), ALU.mult)
        tt_(T(6), T(AI), P("lamre"), ALU.mult)
        tt_(T(7), T(5), P("lamim"), ALU.mult)
        tt_(T(6), T(6), T(7), ALU.subtract)
        tt_(T(FI), T(6), T(4), ALU.mult)
        Acf = sb("Acf", [128, 2, 2, 16], F32)
        for c in range(2):
            op("dve", lambda e, c=c: e.tensor_copy(out=Acf[:, 0, c, :], in_=T(AR)), reads=[s5t], writes=[Acf])
        op("dve", lambda e: e.tensor_copy(out=Acf[:, 1, 0, :], in_=T(AI)), reads=[s5t], writes=[Acf])
        op("dve", lambda e: e.tensor_scalar(out=Acf[:, 1, 1, :], in0=T(AI), scalar1=-1.0, scalar2=None, op0=ALU.mult), reads=[s5t], writes=[Acf])
        tap("Acf", Acf, Acf[:])
        tap("s5t", s5t, s5t[:])
        BD = sb("BD", [128, 2, 96], F32)
        BT = sb("BT", [128, 6, 2, 128], BF16)
        bb1 = sb("bb1", [128, 16], F32)
        bb2 = sb("bb2", [128, 16], F32)
        o_bre, o_bim = offA["bre"][0], offA["bim"][0]
        for gp in range(16):
            bre = PA[:, o_bre + gp * 16:o_bre + gp * 16 + 16]
            bim = PA[:, o_bim + gp * 16:o_bim + gp * 16 + 16]
            fr, fi = s5t[:, FR, gp:gp + 1], s5t[:, FI, gp:gp + 1]
            for g2 in range(2):
                rs = slice(64 * g2, 64 * g2 + 64)
                cs = slice(32 * (gp % 3) + 16 * g2, 32 * (gp % 3) + 16 * g2 + 16)
                op("dve", lambda e, rs=rs, bim=bim, fi=fi: e.tensor_scalar(out=bb1[rs, :], in0=bim[rs, :], scalar1=fi[rs, :], scalar2=None, op0=ALU.mult), reads=[PA, s5t], writes=[bb1])
                op("dve", lambda e, rs=rs, cs=cs, bre=bre, fr=fr: e.scalar_tensor_tensor(out=BD[rs, 0, cs], in0=bre[rs, :], scalar=fr[rs, :], in1=bb1[rs, :], op0=ALU.mult, op1=ALU.subtract), reads=[PA, s5t, bb1], writes=[BD])
                op("dve", lambda e, rs=rs, bre=bre, fi=fi: e.tensor_scalar(out=bb2[rs, :], in0=bre[rs, :], scalar1=fi[rs, :], scalar2=-1.0, op0=ALU.mult, op1=ALU.mult), reads=[PA, s5t], writes=[bb2])
                op("dve", lambda e, rs=rs, cs=cs, bim=bim, fr=fr: e.scalar_tensor_tensor(out=bb1[rs, :], in0=bim[rs, :], scalar=fr[rs, :], in1=bb2[rs, :], op0=ALU.mult, op1=ALU.subtract), reads=[PA, s5t, bb2], writes=[bb1])
                op("dve", lambda e, rs=rs, cs=cs: e.tensor_scalar(out=BD[rs, 1, cs], in0=bb1[rs, :], scalar1=-1.0, scalar2=None, op0=ALU.mult), reads=[bb1], writes=[BD])
            if gp % 3 == 2 or gp == 15:
                tl = gp // 3
                m = 96 if tl < 5 else 32
                pb = bank()
                for w_ in range(2):
                    op("pe", lambda e, pb=pb, w_=w_, m=m: e.transpose(pb[0:m, w_ * 128:(w_ + 1) * 128], BD[:, w_, 0:m], P("ident")), reads=[BD, PA], writes=[pb])
                op("act", lambda e, pb=pb, tl=tl, m=m: e.activation(out=BT[0:m, tl, :, :], in_=pb[0:m, 0:256].rearrange("p (w c) -> p w c", w=2), func=AF.Copy), reads=[pb], writes=[BT])

        tap("BT", BT, BT[:])
        xtok = [sb(f"xtok{i}", [128, D], F32) for i in range(2)]
        tokA = sb("tokA", [128, D], F32)
        tokB = sb("tokB", [128, D], F32)
        sst = sb("sst", [128, 8], F32)
        xnT = sb("xnT", [128, 8, NCOL], BF16)
        Ub = sb("Ub", [128, 6, NB, TC], BF16)
        Sb = sb("Sb", [128, 2, 16, NB, SUB], BF16)
        CTb = sb("CTb", [128, 2, 512], BF16)
        op("act", lambda e: e.activation(out=CTb[:, 0, :], in_=P("ctre"), func=AF.Copy), reads=[PA], writes=[CTb])
        op("act", lambda e: e.activation(out=CTb[:, 1, :], in_=P("ctim"), func=AF.Copy), reads=[PA], writes=[CTb])
        QB = [sb(f"qb{i}", [128, NB, TC + 1], F32) for i in range(14)]
        S = sb("S", [128, 2, 16, NB, SUB + 1], F32)
        stmp = sb("stmp", [128, 2, 2, 16, NB], F32)
        Y = sb("Y", [128, 6, NB, SUB], F32)
        Y1 = sb("Y1", [128, 6, NB, SUB], F32)
        Y2 = Y1
        ZbF = sb("ZbF", [128, 6, NB, TC], BF16)
        SGf = sb("SGf", [128, NB, TC], F32)
        mixin = sb("mixin", [128, 10, NB, TC], BF16)
        ST = [sb(f"ST{j}", [128, NB, 64], F32) for j in range(4)]
        XWA = sb("XWA", [128, NCOL], F32)
        TH = sb("TH", [128, NCOL], F32)
        SGX = sb("SGX", [128, NCOL], F32)
        names = ["qr", "qk", "qv", "sgw", "aa", "gg", "kkt", "t1", "t2", "kap", "kp", "bet", "bon",
                 "cum", "Pm", "Pinv", "oT", "cen"]
        R_ = {n: sb("r_" + n, [128, NCOL], F32) for n in names}
        R_["dd"] = R_["t2"]
        KR = sb("KR", [128, 2, NCOL], BF16)
        OUTS = ["Pm", "bon", "gg"]
        KRs = [KR, sb("KR1", [128, 2, NCOL], BF16)]
        identb = sb("identb", [128, 128], BF16)
        op("act", lambda e: e.activation(out=identb[:], in_=P("ident"), func=AF.Copy), reads=[PA], writes=[identb])
        STb = [sb(f"STb{j}", [128, NB, 64], BF16) for j in range(4)]
        RS13, RS48 = [], []
        t1b = sb("r_t1b", [128, NCOL], F32)
        t2b = sb("r_t2b", [128, NCOL], F32)
        for par in range(2):
            d13 = dict(R_)
            if par == 1:
                for n_ in OUTS:
                    d13[n_] = sb("r1_" + n_, [128, NCOL], F32)
            d13["lw"] = d13["sgw"]
            d13["Ppv"] = d13["t2"]
            d13["bt"] = sb(f"r{par}_btb", [128, NCOL], BF16)
            d13["kt"] = sb(f"r{par}_ktb", [128, NCOL], BF16)
            d13["qvb"] = sb(f"r{par}_qvb", [128, NCOL], BF16)
            RS13.append(d13)
            d48 = {n_: d13[n_] for n_ in OUTS}
            d48["bt"] = d13["bt"]
            d48["kt"] = d13["kt"]
            d48["qvb"] = d13["qvb"]
            d48["oT"] = R_["oT"]
            d48["cen"] = R_["cen"]
            d48["t1"] = t1b
            d48["t2"] = t2b
            RS48.append(d48)
        Vt = sb("Vt", [128, NB, 64], BF16)
        Btk = sb("Btk", [128, NB, 64], BF16)
        Ktk = sb("Ktk", [128, NB, 64], BF16)
        GbS = sb("GbS", [128, NB, 128], BF16)
        GkS = sb("GkS", [128, NB, 128], BF16)
        Xa = [sb(f"Xa{i}", [128, NB, 64], BF16) for i in range(2)]
        XTa = [sb(f"XTa{i}", [128, NB, 64], BF16) for i in range(2)]
        Wt = sb("Wt", [128, NB, 64], F32)
        Wtb = sb("Wtb", [128, NB, 64], BF16)
        Rn = sb("Rn", [128, NB, 64], BF16)
        Uu = sb("Uu", [128, NB, 64], BF16)
        omka = sb("omka", [128, 4], F32)

        op("dve", lambda e: e.tensor_scalar(out=omka[:], in0=P("ka"), scalar1=-1.0, scalar2=1.0, op0=ALU.mult, op1=ALU.add), reads=[PA], writes=[omka])

        def rstd_from_ss(col):
            op("dve", lambda e: e.tensor_scalar(out=sst[:, col:col + 1], in0=sst[:, col:col + 1], scalar1=1.0 / D, scalar2=1e-6, op0=ALU.mult, op1=ALU.add), reads=[sst], writes=[sst])
            op("act", lambda e: e.activation(out=sst[:, col:col + 1], in_=sst[:, col:col + 1], func=AF.Sqrt), reads=[sst], writes=[sst])
            op("dve", lambda e: e.reciprocal(out=sst[:, col:col + 1], in_=sst[:, col:col + 1]), reads=[sst], writes=[sst])

        ident = P("ident")
        onesblk = P("onesblk")

        for it in range(nit):
            t0 = it * TC
            for tt in range(2):
                for b2 in range(2):
                    dma("sp", xtok[tt], xtok[tt][64 * b2:64 * b2 + 64, :], x[2 * tt + b2, t0:t0 + TC, :])
            for tt in range(2):
                op("dve", lambda e, tt=tt: e.scalar_tensor_tensor(out=tokA[:], in0=xtok[tt][:], scalar=1.0, in1=xtok[tt][:], op0=ALU.mult, op1=ALU.mult, accum_out=sst[:, tt:tt + 1]), reads=[xtok[tt]], writes=[tokA, sst])
                rstd_from_ss(tt)
                op("dve", lambda e, tt=tt: e.tensor_scalar(out=tokA[:], in0=xtok[tt][:], scalar1=sst[:, tt:tt + 1], scalar2=None, op0=ALU.mult), reads=[xtok[tt], sst], writes=[tokA])
                for kh in range(2):
                    pb = bank()
                    for k4 in range(4):
                        k = kh * 4 + k4
                        op("pe", lambda e, pb=pb, k=k, k4=k4: e.transpose(pb[:, k4 * 128:(k4 + 1) * 128], tokA[:, k * 128:(k + 1) * 128], ident), reads=[tokA, PA], writes=[pb])
                    gpre = P("gpre", kh * 4, kh * 4 + 4).unsqueeze(2).to_broadcast([128, 4, 128])
                    op("dve", lambda e, pb=pb, kh=kh, tt=tt, gpre=gpre: e.tensor_tensor(out=xnT[:, kh * 4:kh * 4 + 4, tt * 128:(tt + 1) * 128], in0=pb[:, :].rearrange("p (k c) -> p k c", k=4), in1=gpre, op=ALU.mult), reads=[pb, PA], writes=[xnT])
            for ct in range(20):
                pb = bank()
                if ct < 6:
                    c0, m = 96 * ct, TR[ct]
                else:
                    c0, m = 512 + 128 * (ct - 6), 128
                for k in range(8):
                    op("pe", lambda e, pb=pb, k=k, c0=c0, m=m: e.matmul(pb[0:m, 0:NCOL], lhsT=Win[:, k, c0:c0 + m], rhs=xnT[:, k, :], start=(k == 0), stop=(k == 7)), reads=[Win, xnT], writes=[pb])
                if ct < 6:
                    op("act", lambda e, pb=pb, ct=ct, m=m: e.activation(out=Ub[0:m, ct, :, :], in_=pb[0:m, 0:NCOL].rearrange("p (b t) -> p b t", b=NB), func=AF.Copy), reads=[pb], writes=[Ub])
                else:
                    qb = QB[ct - 6]
                    op("act", lambda e, pb=pb, qb=qb: e.activation(out=qb[:, :, 1:TC + 1], in_=pb[:, 0:NCOL].rearrange("p (b t) -> p b t", b=NB), func=AF.Copy), reads=[pb], writes=[qb])

            def s5_front(sc):
                tsl = slice(sc * SUB, (sc + 1) * SUB)
                for gp in range(16):
                    tl, q = gp // 3, gp % 3
                    pb = bank()
                    for w_ in range(2):
                        op("pe", lambda e, pb=pb, tl=tl, q=q, w_=w_: e.matmul(pb[:, w_ * NB * SUB:(w_ + 1) * NB * SUB], lhsT=BT[32 * q:32 * q + 32, tl, w_, :], rhs=Ub[32 * q:32 * q + 32, tl, :, tsl], start=True, stop=True), reads=[BT, Ub], writes=[pb])
                    op("act", lambda e, pb=pb, gp=gp: e.activation(out=S[:, :, gp, :, 1:SUB + 1], in_=pb[:, 0:2 * NB * SUB].rearrange("p (c b t) -> p c b t", c=2, b=NB), func=AF.Copy), reads=[pb], writes=[S])
                for t in range(SUB):
                    prev = S[:, :, :, :, t]
                    cur = S[:, :, :, :, t + 1]
                    a1 = Acf[:, 0, :, :].unsqueeze(3).to_broadcast([128, 2, 16, NB])
                    op(SCAN_ENG, lambda e, prev=prev, a1=a1: e.tensor_tensor(out=stmp[:, 0, :, :, :], in0=prev, in1=a1, op=ALU.mult), reads=[S, Acf], writes=[stmp])
                    for c in range(2):
                        a2 = Acf[:, 1, c, :].unsqueeze(2).to_broadcast([128, 16, NB])
                        op(SCAN_ENG, lambda e, c=c, a2=a2, t=t: e.tensor_tensor(out=stmp[:, 1, c, :, :], in0=S[:, 1 - c, :, :, t], in1=a2, op=ALU.mult), reads=[S, Acf], writes=[stmp])
                    op(SCAN_ENG, lambda e: e.tensor_tensor(out=stmp[:, 0, :, :, :], in0=stmp[:, 0, :, :, :], in1=stmp[:, 1, :, :, :], op=ALU.add), reads=[stmp], writes=[stmp])
                    op(SCAN_ENG, lambda e, cur=cur: e.tensor_tensor(out=cur, in0=cur, in1=stmp[:, 0, :, :, :], op=ALU.add), reads=[stmp, S], writes=[S])
                tap("S", S, S[:])

            def s5_back(sc):
                tsl = slice(sc * SUB, (sc + 1) * SUB)
                op("act", lambda e: e.activation(out=Sb[:, :, :, :, :], in_=S[:, :, :, :, 1:SUB + 1], func=AF.Copy), reads=[S], writes=[Sb])
                for tl in range(6):
                    pb = bank()
                    for q in range(3 if tl < 5 else 1):
                        gp = tl * 3 + q
                        oc, nct = offA["ctre"][0], offA["ctim"][0]
                        op("pe", lambda e, pb=pb, q=q, gp=gp, oc=oc: e.matmul(pb[32 * q:32 * q + 32, 0:NB * SUB], lhsT=CTb[:, 0, gp * 32:gp * 32 + 32], rhs=Sb[:, 0, gp, :, :], start=True, stop=False), reads=[CTb, Sb], writes=[pb])
                        op("pe", lambda e, pb=pb, q=q, gp=gp, nct=nct: e.matmul(pb[32 * q:32 * q + 32, 0:NB * SUB], lhsT=CTb[:, 1, gp * 32:gp * 32 + 32], rhs=Sb[:, 1, gp, :, :], start=False, stop=True), reads=[CTb, Sb], writes=[pb])
                    op("dve", lambda e, pb=pb, tl=tl: e.scalar_tensor_tensor(out=Y[:, tl, :, :], in0=Ub[:, tl, :, tsl], scalar=P("dskip", tl, tl + 1), in1=pb[:, 0:NB * SUB].rearrange("p (b t) -> p b t", b=NB), op0=ALU.mult, op1=ALU.add), reads=[Ub, PA, pb], writes=[Y])
                tap("Y", Y, Y[:])
                op(SCAN_ENG, lambda e: e.tensor_copy(out=S[:, :, :, :, 0], in_=S[:, :, :, :, SUB]), reads=[S], writes=[S])
                fl = lambda b_: b_[:].rearrange("p a b t -> p (a b t)")
                op("dve", lambda e: e.tensor_tensor(out=fl(Y1), in0=fl(Y), in1=fl(Y), op=ALU.mult), reads=[Y], writes=[Y1])
                op("dve", lambda e: e.tensor_scalar(out=fl(Y1), in0=fl(Y1), scalar1=0.044715, scalar2=1.0, op0=ALU.mult, op1=ALU.add), reads=[Y1], writes=[Y1])
                op("dve", lambda e: e.tensor_tensor(out=fl(Y1), in0=fl(Y1), in1=fl(Y), op=ALU.mult), reads=[Y, Y1], writes=[Y1])
                op("act", lambda e: e.activation(out=fl(Y2), in_=fl(Y1), func=AF.Sigmoid, scale=1.5957691216057308), reads=[Y1], writes=[Y2])
                op("dve", lambda e: e.tensor_tensor(out=ZbF[:, :, :, tsl], in0=Y[:, :, :, :], in1=Y2[:, :, :, :], op=ALU.mult), reads=[Y, Y2], writes=[ZbF])
                if sc != TC // SUB - 1:
                    return
                for ct in range(6):
                    pb = bank()
                    m = TR[ct]
                    for k in range(6):
                        op("pe", lambda e, pb=pb, k=k, ct=ct, m=m: e.matmul(pb[0:m, 0:NCOL], lhsT=Wglu[0:TR[k], k, ct * 96:ct * 96 + m], rhs=ZbF[0:TR[k], k, :, :], start=(k == 0), stop=(k == 5)), reads=[Wglu, ZbF], writes=[pb])
                    op("act", lambda e, pb=pb, ct=ct, m=m: e.activation(out=SGf[0:m, :, :], in_=pb[0:m, 0:NCOL].rearrange("p (b t) -> p b t", b=NB), func=AF.Sigmoid, bias=P("bglu", ct, ct + 1, rows=slice(0, m))), reads=[pb, PA], writes=[SGf])
                    op("dve", lambda e, ct=ct, m=m: e.tensor_tensor(out=mixin[0:m, ct, :, :], in0=ZbF[0:m, ct, :, :], in1=SGf[0:m, :, :], op=ALU.mult), reads=[ZbF, SGf], writes=[mixin])

            def shift(dst, qb, mucol):
                v3 = lambda b_: b_[:].rearrange("p (b t) -> p b t", b=NB)
                op("dve", lambda e: e.tensor_tensor(out=v3(R_["dd"]), in0=qb[:, :, 0:TC], in1=qb[:, :, 1:TC + 1], op=ALU.subtract), reads=[qb], writes=[R_["dd"]])
                op("dve", lambda e: e.scalar_tensor_tensor(out=v3(dst), in0=v3(R_["dd"]), scalar=P("mu", mucol, mucol + 1), in1=qb[:, :, 1:TC + 1], op0=ALU.mult, op1=ALU.add), reads=[R_["dd"], qb, PA], writes=[dst])
                op("dve", lambda e: e.tensor_copy(out=qb[:, :, 0:1], in_=qb[:, :, TC:TC + 1]), reads=[qb], writes=[qb])

            shift(XWA, QB[12], 12)
            op("act", lambda e: e.activation(out=TH[0:64, :], in_=XWA[0:64, :], func=AF.Tanh), reads=[XWA], writes=[TH])
            shift(SGX, QB[13], 13)
            op("act", lambda e: e.activation(out=SGX[:], in_=SGX[:], func=AF.Sigmoid), reads=[SGX], writes=[SGX])
            tt2 = lambda o, a, b, f, eng="dve": op(eng, lambda e: e.tensor_tensor(out=o[:], in0=a[:], in1=b[:], op=f), reads=[a, b], writes=[o])
            def rwkv_s13(j):
                r = RS13[j % 2]
                KR = KRs[j % 2]
                shift(r["qr"], QB[j], j)
                shift(r["qk"], QB[4 + j], 4 + j)
                shift(r["qv"], QB[8 + j], 8 + j)
                op("act", lambda e: e.activation(out=r["qvb"][:], in_=r["qv"][:], func=AF.Copy), reads=[r["qv"]], writes=[r["qvb"]])
                yield
                cs = slice(j * 128, (j + 1) * 128)
                pbw = bank()
                pba = bank()
                op("pe", lambda e, pbw=pbw, cs=cs: e.matmul(pbw[:, 0:NCOL], lhsT=P("wau", rows=slice(0, 64))[:, cs], rhs=TH[0:64, :], start=True, stop=True), reads=[PA, TH], writes=[pbw])
                op("pe", lambda e, pba=pba, cs=cs: e.matmul(pba[:, 0:NCOL], lhsT=P("wau", rows=slice(64, 128))[:, cs], rhs=XWA[64:128, :], start=True, stop=True), reads=[PA, XWA], writes=[pba])
                op("act", lambda e, pbw=pbw, j=j: e.activation(out=r["sgw"][:], in_=pbw[:, 0:NCOL], func=AF.Sigmoid, bias=P("w0", j, j + 1)), reads=[pbw, PA], writes=[r["sgw"]])
                op("act", lambda e, pba=pba, j=j: e.activation(out=r["aa"][:], in_=pba[:, 0:NCOL], func=AF.Sigmoid, bias=P("a0", j, j + 1)), reads=[pba, PA], writes=[r["aa"]])
                pb = bank()
                op("pe", lambda e, pb=pb, cs=cs: e.matmul(pb[:, 0:NCOL], lhsT=P("gup")[:, cs], rhs=SGX[:], start=True, stop=True), reads=[PA, SGX], writes=[pb])
                op("act", lambda e, pb=pb: e.activation(out=r["gg"][:], in_=pb[:, 0:NCOL], func=AF.Copy), reads=[pb], writes=[r["gg"]])
                yield
                if KSTOP <= 1:
                    return
                op("dve", lambda e: e.tensor_scalar(out=r["lw"][:], in0=r["sgw"][:], scalar1=-0.6065306597126334, scalar2=None, op0=ALU.mult), reads=[r["sgw"]], writes=[r["lw"]])
                op("dve", lambda e, j=j: e.tensor_scalar(out=r["kkt"][:], in0=r["qk"][:], scalar1=P("kk", j, j + 1), scalar2=None, op0=ALU.mult), reads=[r["qk"], PA], writes=[r["kkt"]])
                tt2(r["t1"], r["kkt"], r["kkt"], ALU.mult)
                pb = bank()
                op("pe", lambda e, pb=pb: e.matmul(pb[:, 0:NCOL], lhsT=onesblk, rhs=r["t1"][:], start=True, stop=True), reads=[PA, r["t1"]], writes=[pb])
                op("dve", lambda e, pb=pb: e.tensor_scalar(out=r["t2"][:], in0=pb[:, 0:NCOL], scalar1=1e-24, scalar2=None, op0=ALU.max), reads=[pb], writes=[r["t2"]])
                op("act", lambda e: e.activation(out=r["t2"][:], in_=r["t2"][:], func=AF.Sqrt), reads=[r["t2"]], writes=[r["t2"]])
                op("dve", lambda e: e.reciprocal(out=r["t2"][:], in_=r["t2"][:]), reads=[r["t2"]], writes=[r["t2"]])
                tt2(r["kap"], r["kkt"], r["t2"], ALU.mult)
                yield
                op("dve", lambda e, j=j: e.tensor_scalar(out=r["t1"][:], in0=r["aa"][:], scalar1=P("ka", j, j + 1), scalar2=omka[:, j:j + 1], op0=ALU.mult, op1=ALU.add), reads=[r["aa"], PA, omka], writes=[r["t1"]])
                tt2(r["kp"], r["qk"], r["t1"], ALU.mult)
                tt2(r["bet"], r["kap"], r["aa"], ALU.mult)
                op("dve", lambda e, j=j: e.scalar_tensor_tensor(out=r["t1"][:], in0=r["qr"][:], scalar=P("rk", j, j + 1), in1=r["kp"][:], op0=ALU.mult, op1=ALU.mult), reads=[r["qr"], r["kp"], PA], writes=[r["t1"]])
                pb = bank()
                op("pe", lambda e, pb=pb: e.matmul(pb[:, 0:NCOL], lhsT=onesblk, rhs=r["t1"][:], start=True, stop=True), reads=[PA, r["t1"]], writes=[pb])
                op("dve", lambda e, pb=pb: e.tensor_tensor(out=r["bon"][:], in0=pb[:, 0:NCOL], in1=r["qv"][:], op=ALU.mult), reads=[pb, r["qv"]], writes=[r["bon"]])
                yield
                if KSTOP <= 2:
                    return
                op("dve", lambda e: e.tensor_tensor_scan(out=r["cum"][:], data0=P("scanmask"), data1=r["lw"][:], initial=0.0, op0=ALU.mult, op1=ALU.add), reads=[PA, r["lw"]], writes=[r["cum"]])
                op("act", lambda e: e.activation(out=r["Pm"][:], in_=r["cum"][:], func=AF.Exp), reads=[r["cum"]], writes=[r["Pm"]])
                op("act", lambda e: e.activation(out=r["Pinv"][:], in_=r["cum"][:], func=AF.Exp, scale=-1.0), reads=[r["cum"]], writes=[r["Pinv"]])
                tt2(r["t2"], r["cum"], r["lw"], ALU.subtract)
                op("act", lambda e: e.activation(out=r["Ppv"][:], in_=r["t2"][:], func=AF.Exp), reads=[r["t2"]], writes=[r["Ppv"]])
                op("dve", lambda e: e.tensor_tensor(out=KR[:, 0, :], in0=r["kap"][:], in1=r["Ppv"][:], op=ALU.mult), reads=[r["kap"], r["Ppv"]], writes=[KR])
                op("dve", lambda e: e.tensor_tensor(out=KR[:, 1, :], in0=r["qr"][:], in1=r["Pm"][:], op=ALU.mult), reads=[r["qr"], r["Pm"], KR], writes=[KR])
                tt2(r["bt"], r["bet"], r["Pinv"], ALU.mult)
                tt2(r["kt"], r["kp"], r["Pinv"], ALU.mult)
                yield
                if KSTOP <= 3:
                    return
                yield

            def rwkv_s48(j):
                r = RS48[j % 2]
                KR = KRs[j % 2]
                HS = [slice(0, 64), slice(64, 128)]
                f2 = lambda b_, rs: b_[rs, :, :].rearrange("p b c -> p (b c)")
                idb = [identb[HS[h2], 64 * h2:64 * h2 + 64] for h2 in range(2)]
                BCS = [slice(b * 64, (b + 1) * 64) for b in range(NB)]
                pv = [bank(), bank()]
                pk2 = [bank(), bank()]
                for b in range(NB):
                    bc = BCS[b]
                    for h2 in range(2):
                        rs = HS[h2]
                        op("pe", lambda e: e.matmul(pv[h2][rs, b * 64:(b + 1) * 64], lhsT=r["qvb"][rs, bc], rhs=idb[h2], start=True, stop=True), reads=[r["qvb"], identb], writes=[pv[h2]])
                        op("pe", lambda e: e.matmul(pv[h2][rs, 256 + b * 64:256 + (b + 1) * 64], lhsT=r["bt"][rs, bc], rhs=idb[h2], start=True, stop=True), reads=[r["bt"], identb], writes=[pv[h2]])
                        op("pe", lambda e: e.matmul(pk2[h2][rs, b * 64:(b + 1) * 64], lhsT=r["kt"][rs, bc], rhs=idb[h2], start=True, stop=True), reads=[r["kt"], identb], writes=[pk2[h2]])
                for h2 in range(2):
                    rs = HS[h2]
                    op("act", lambda e: e.activation(out=f2(Vt, rs), in_=pv[h2][rs, 0:256], func=AF.Copy), reads=[pv[h2]], writes=[Vt])
                    op("act", lambda e: e.activation(out=f2(Btk, rs), in_=pv[h2][rs, 256:512], func=AF.Copy), reads=[pv[h2]], writes=[Btk])
                    op("act", lambda e: e.activation(out=f2(Ktk, rs), in_=pk2[h2][rs, 0:256], func=AF.Copy), reads=[pk2[h2]], writes=[Ktk])
                yield
                if KSTOP <= 4:
                    return
                pgb = [bank(), bank()]
                pgk = [bank(), bank()]
                pl = [bank(), bank()]
                for b in range(NB):
                    bc = BCS[b]
                    for h2 in range(2):
                        rs = HS[h2]
                        op("pe", lambda e: e.matmul(pl[h2][rs, b * 64:(b + 1) * 64], lhsT=KR[rs, 0, bc], rhs=r["bt"][rs, bc], start=True, stop=True), reads=[r["bt"], KR], writes=[pl[h2]])
                        op("pe", lambda e: e.matmul(pgb[h2][rs, b * 128:(b + 1) * 128], lhsT=r["bt"][rs, bc], rhs=KR[rs, :, bc], start=True, stop=True), reads=[r["bt"], KR], writes=[pgb[h2]])
                        op("pe", lambda e: e.matmul(pgk[h2][rs, b * 128:(b + 1) * 128], lhsT=r["kt"][rs, bc], rhs=KR[rs, :, bc], start=True, stop=True), reads=[r["kt"], KR], writes=[pgk[h2]])
                for h2 in range(2):
                    rs = HS[h2]
                    mG = P("maskG", rows=rs).unsqueeze(1).to_broadcast([64, NB, 128])
                    mL = P("maskL", rows=rs).unsqueeze(1).to_broadcast([64, NB, 64])
                    i64 = P("ident", 64 * h2, 64 * h2 + 64, rows=rs).unsqueeze(1).to_broadcast([64, NB, 64])
                    op("dve", lambda e: e.tensor_tensor(out=Xa[0][rs, :, :], in0=pl[h2][rs, 0:256].rearrange("p (u c) -> p u c", u=NB), in1=mL, op=ALU.mult), reads=[pl[h2], PA], writes=[Xa[0]])
                    op("dve", lambda e: e.tensor_tensor(out=GbS[rs, :, :], in0=pgb[h2][rs, :].rearrange("p (u c) -> p u c", u=NB), in1=mG, op=ALU.mult), reads=[pgb[h2], PA], writes=[GbS])
                    op("dve", lambda e: e.tensor_tensor(out=Wt[rs, :, :], in0=i64, in1=GbS[rs, :, 0:64], op=ALU.subtract), reads=[PA, GbS], writes=[Wt])
                    op("act", lambda e: e.activation(out=XTa[0][rs, :, :], in_=GbS[rs, :, 0:64], func=AF.Copy), reads=[GbS], writes=[XTa[0]])
                    op("act", lambda e: e.activation(out=Wtb[rs, :, :], in_=Wt[rs, :, :], func=AF.Copy), reads=[Wt], writes=[Wtb])
                    op("dve", lambda e: e.tensor_tensor(out=GkS[rs, :, :], in0=pgk[h2][rs, :].rearrange("p (u c) -> p u c", u=NB), in1=mG, op=ALU.mult), reads=[pgk[h2], PA], writes=[GkS])
                yield
                if KSTOP <= 5:
                    return
                for lvl in range(1, 6):
                    Xp = Xa[(lvl - 1) % 2]
                    Xn = Xa[lvl % 2]
                    XTn = XTa[lvl % 2]
                    XTp_buf, XTp_ap = XTa[(lvl - 1) % 2], (lambda rs, b, t_=XTa[(lvl - 1) % 2]: t_[rs, b, :])
                    px = [bank(), bank()]
                    for b in range(NB):
                        for h2 in range(2):
                            rs = HS[h2]
                            op("pe", lambda e: e.matmul(px[h2][rs, b * 64:(b + 1) * 64], lhsT=XTp_ap(rs, b), rhs=Xp[rs, b, :], start=True, stop=True), reads=[Xp, XTp_buf], writes=[px[h2]])
                    for h2 in range(2):
                        rs = HS[h2]
                        op("act", lambda e: e.activation(out=f2(Xn, rs), in_=px[h2][rs, 0:256], func=AF.Copy), reads=[px[h2]], writes=[Xn])
                    if lvl < 5:
                        pxt = [bank(), bank()]
                        for b in range(NB):
                            for h2 in range(2):
                                rs = HS[h2]
                                op("pe", lambda e: e.matmul(pxt[h2][rs, b * 64:(b + 1) * 64], lhsT=Xp[rs, b, :], rhs=XTp_ap(rs, b), start=True, stop=True), reads=[Xp, XTp_buf], writes=[pxt[h2]])
                        for h2 in range(2):
                            rs = HS[h2]
                            op("act", lambda e: e.activation(out=f2(XTn, rs), in_=pxt[h2][rs, 0:256], func=AF.Copy), reads=[pxt[h2]], writes=[XTn])
                    pw = [bank(), bank()]
                    for b in range(NB):
                        for h2 in range(2):
                            rs = HS[h2]
                            op("pe", lambda e: e.matmul(pw[h2][rs, b * 64:(b + 1) * 64], lhsT=Xn[rs, b, :], rhs=Wtb[rs, b, :], start=True, stop=True), reads=[Xn, Wtb], writes=[pw[h2]])
                    for h2 in range(2):
                        rs = HS[h2]
                        op("dve", lambda e: e.tensor_tensor(out=f2(Wt, rs), in0=pw[h2][rs, 0:256], in1=f2(Wt, rs), op=ALU.add), reads=[pw[h2], Wt], writes=[Wt])
                        op("act", lambda e: e.activation(out=f2(Wtb, rs), in_=f2(Wt, rs), func=AF.Copy), reads=[Wt], writes=[Wtb])
                    yield
                yield
                if KSTOP <= 6:
                    return
                STj = ST[j]
                pr = [bank(), bank()]
                for b in range(NB):
                    bc = BCS[b]
                    for h2 in range(2):
                        rs = HS[h2]
                        op("pe", lambda e: e.matmul(pr[h2][rs, b * 64:(b + 1) * 64], lhsT=KR[rs, 0, bc], rhs=STb[j][rs, b, :], start=True, stop=False), reads=[KR, STb[j]], writes=[pr[h2]])
                        op("pe", lambda e: e.matmul(pr[h2][rs, b * 64:(b + 1) * 64], lhsT=GkS[rs, b, 0:64], rhs=Vt[rs, b, :], start=False, stop=True), reads=[GkS, Vt], writes=[pr[h2]])
                for h2 in range(2):
                    rs = HS[h2]
                    op("act", lambda e: e.activation(out=f2(Rn, rs), in_=pr[h2][rs, 0:256], func=AF.Copy, scale=-1.0), reads=[pr[h2]], writes=[Rn])
                pu = [bank(), bank()]
                for b in range(NB):
                    for h2 in range(2):
                        rs = HS[h2]
                        op("pe", lambda e: e.matmul(pu[h2][rs, b * 64:(b + 1) * 64], lhsT=Wtb[rs, b, :], rhs=Rn[rs, b, :], start=True, stop=True), reads=[Wtb, Rn], writes=[pu[h2]])
                for h2 in range(2):
                    rs = HS[h2]
                    op("act", lambda e: e.activation(out=f2(Uu, rs), in_=pu[h2][rs, 0:256], func=AF.Copy), reads=[pu[h2]], writes=[Uu])
                yield
                if KSTOP <= 7:
                    return
                po = [bank(), bank()]
                psn = [bank(), bank()]
                for b in range(NB):
                    bc = BCS[b]
                    for h2 in range(2):
                        rs = HS[h2]
                        op("pe", lambda e: e.matmul(po[h2][rs, bc], lhsT=STb[j][rs, b, :], rhs=KR[rs, 1, bc], start=True, stop=False), reads=[STb[j], KR], writes=[po[h2]])
                        op("pe", lambda e: e.matmul(po[h2][rs, bc], lhsT=Uu[rs, b, :], rhs=GbS[rs, b, 64:128], start=False, stop=False), reads=[Uu, GbS], writes=[po[h2]])
                        op("pe", lambda e: e.matmul(po[h2][rs, bc], lhsT=Vt[rs, b, :], rhs=GkS[rs, b, 64:128], start=False, stop=True), reads=[Vt, GkS], writes=[po[h2]])
                        op("pe", lambda e: e.matmul(psn[h2][rs, bc], lhsT=Btk[rs, b, :], rhs=Uu[rs, b, :], start=True, stop=False), reads=[Btk, Uu], writes=[psn[h2]])
                        op("pe", lambda e: e.matmul(psn[h2][rs, bc], lhsT=Ktk[rs, b, :], rhs=Vt[rs, b, :], start=False, stop=True), reads=[Ktk, Vt], writes=[psn[h2]])
                for h2 in range(2):
                    rs = HS[h2]
                    op("act", lambda e: e.activation(out=r["oT"][rs, :], in_=po[h2][rs, 0:NCOL], func=AF.Copy), reads=[po[h2]], writes=[r["oT"]])
                    op("dve", lambda e: e.tensor_tensor(out=r["t1"][rs, :], in0=psn[h2][rs, 0:NCOL], in1=f2(STj, rs), op=ALU.add), reads=[psn[h2], STj], writes=[r["t1"]])
                    pT = r["Pm"][rs, :].rearrange("p (b t) -> p b t", b=NB)[:, :, TC - 1:TC].to_broadcast([64, NB, 64])
                    op("dve", lambda e: e.tensor_tensor(out=STj[rs, :, :], in0=r["t1"][rs, :].rearrange("p (b c) -> p b c", b=NB), in1=pT, op=ALU.mult), reads=[r["t1"], r["Pm"]], writes=[STj])
                    op("act", lambda e: e.activation(out=STb[j][rs, :, :], in_=STj[rs, :, :], func=AF.Copy), reads=[STj], writes=[STb[j]])
                yield
                if KSTOP <= 8:
                    return
                pb = bank()
                op("pe", lambda e, pb=pb: e.matmul(pb[:, 0:NCOL], lhsT=onesblk, rhs=r["oT"][:], start=True, stop=True), reads=[PA, r["oT"]], writes=[pb])
                op("dve", lambda e, pb=pb: e.scalar_tensor_tensor(out=r["cen"][:], in0=pb[:, 0:NCOL], scalar=-1.0 / 64, in1=r["oT"][:], op0=ALU.mult, op1=ALU.add), reads=[pb, r["oT"]], writes=[r["cen"]])
                tt2(r["t1"], r["cen"], r["cen"], ALU.mult)
                pb = bank()
                op("pe", lambda e, pb=pb: e.matmul(pb[:, 0:NCOL], lhsT=onesblk, rhs=r["t1"][:], start=True, stop=True), reads=[PA, r["t1"]], writes=[pb])
                op("dve", lambda e, pb=pb: e.tensor_scalar(out=r["t2"][:], in0=pb[:, 0:NCOL], scalar1=1.0 / 64, scalar2=GN_EPS, op0=ALU.mult, op1=ALU.add), reads=[pb], writes=[r["t2"]])
                op("act", lambda e: e.activation(out=r["t2"][:], in_=r["t2"][:], func=AF.Sqrt), reads=[r["t2"]], writes=[r["t2"]])
                op("dve", lambda e: e.reciprocal(out=r["t2"][:], in_=r["t2"][:]), reads=[r["t2"]], writes=[r["t2"]])
                tt2(r["cen"], r["cen"], r["t2"], ALU.mult)
                op("dve", lambda e, j=j: e.tensor_scalar(out=r["cen"][:], in0=r["cen"][:], scalar1=P("gnw", j, j + 1), scalar2=P("gnb", j, j + 1), op0=ALU.mult, op1=ALU.add), reads=[r["cen"], PA], writes=[r["cen"]])
                tt2(r["cen"], r["cen"], r["bon"], ALU.add)
                op("dve", lambda e, j=j: e.tensor_tensor(out=mixin[:, 6 + j, :, :].rearrange("p b t -> p (b t)"), in0=r["cen"][:], in1=r["gg"][:], op=ALU.mult), reads=[r["cen"], r["gg"]], writes=[mixin])

            n_s5 = 0 if "s" in SKIP else TC // SUB
            n_rw = 0 if "r" in SKIP else 4

            def run_all(g):
                for _ in g:
                    pass

            def zipgen(ga, gb):
                alive = [ga, gb]
                while alive:
                    for g in list(alive):
                        try:
                            next(g)
                        except StopIteration:
                            alive.remove(g)

            if n_rw:
                run_all(rwkv_s13(0))
            for i_ in range(4):
                if i_ < n_s5:
                    s5_front(i_)
                if i_ < n_rw:
                    if i_ + 1 < n_rw:
                        zipgen(rwkv_s48(i_), rwkv_s13(i_ + 1))
                    else:
                        run_all(rwkv_s48(i_))
                if i_ < n_s5:
                    s5_back(i_)
            tap("mixs5", mixin, mixin[:, 0:6, :, :])

            for tt in range(2):
                for dh in range(2):
                    pb = bank()
                    for k in range(10):
                        kr = TR[k] if k < 6 else 128
                        op("pe", lambda e, pb=pb, k=k, kr=kr, tt=tt, dh=dh: e.matmul(pb[:, :], lhsT=mixin[0:kr, k, 2 * tt:2 * tt + 2, :], rhs=Wout[0:kr, k, dh * 512:(dh + 1) * 512], start=(k == 0), stop=(k == 9)), reads=[Wout, mixin], writes=[pb])
                    if dh == 0:
                        op("dve", lambda e, pb=pb, dh=dh: e.tensor_copy(out=tokB[:, dh * 512:(dh + 1) * 512], in_=pb[:, :]), reads=[pb], writes=[tokB])
                    else:
                        op("act", lambda e, pb=pb, dh=dh: e.activation(out=tokB[:, dh * 512:(dh + 1) * 512], in_=pb[:, :], func=AF.Copy), reads=[pb], writes=[tokB])
                op("dve", lambda e, tt=tt: e.scalar_tensor_tensor(out=tokA[:], in0=tokB[:], scalar=1.0, in1=tokB[:], op0=ALU.mult, op1=ALU.mult, accum_out=sst[:, 2 + tt:3 + tt]), reads=[tokB], writes=[tokA, sst])
                rstd_from_ss(2 + tt)
                op("dve", lambda e, tt=tt: e.scalar_tensor_tensor(out=tokA[:], in0=tokB[:], scalar=sst[:, 2 + tt:3 + tt], in1=P("gBmix"), op0=ALU.mult, op1=ALU.mult), reads=[tokB, sst, PA], writes=[tokA])
                op("dve", lambda e, tt=tt: e.tensor_tensor(out=tokA[:], in0=tokA[:], in1=xtok[tt][:], op=ALU.add), reads=[tokA, xtok[tt]], writes=[tokA])
                for b2 in range(2):
                    dma("sp", None, hsc[2 * tt + b2, t0:t0 + TC, :], tokA[64 * b2:64 * b2 + 64, :], reads=[tokA], out_final=True)
        fw.emit()
    if not do_ffn:
        fw.stack.close()
        return nc

    fw2 = fw
    fw2.final_tokens = []
    with fw2.tscope():
        sb, op, dma = fw2.sb, fw2.op, fw2.dma
        PB = sb("PB", [128, nB], F32)

        def P2(name, c0=0, c1=None):
            o, n = offB[name]
            c1 = n if c1 is None else c1
            return PB[:, o + c0:o + c1]
        Wup = sb("Wup", [128, 8, DFF], BF16)
        Wdn = sb("Wdn", [128, 32, D], BF16)
        banks = [fw2.ps(f"bankb{i}", [128, 512], F32) for i in range(8)]
        bi = [0]

        def bank():
            b = banks[bi[0] % 8]
            bi[0] += 1
            return b
        dma("sp", PB, PB[:], ppb)
        for k in range(8):
            for h in range(2):
                dma("pool", Wup, Wup[:, k, h * 2048:(h + 1) * 2048], w_up[k * 128:(k + 1) * 128, h * 2048:(h + 1) * 2048])
        for k in range(32):
            dma("pool", Wdn, Wdn[:, k, :], w_dn[k * 128:(k + 1) * 128, :])
        htok = [sb(f"htok{i}", [128, D], F32) for i in range(2)]
        tokA = sb("tokA2", [128, D], F32)
        tokB = sb("tokB2", [128, D], F32)
        sst = sb("sst2", [128, 8], F32)
        hnT = sb("hnT", [128, 8, NCOL], BF16)
        hid = sb("hid", [128, 32, NCOL], BF16)
        rl = [sb(f"rl{i}", [128, NCOL], F32) for i in range(2)]
        ffT = sb("ffT", [128, 8, NCOL], F32)
        ident = P2("ident")

        def rstd_from_ss2(col):
            op("dve", lambda e: e.tensor_scalar(out=sst[:, col:col + 1], in0=sst[:, col:col + 1], scalar1=1.0 / D, scalar2=1e-6, op0=ALU.mult, op1=ALU.add), reads=[sst], writes=[sst])
            op("act", lambda e: e.activation(out=sst[:, col:col + 1], in_=sst[:, col:col + 1], func=AF.Sqrt), reads=[sst], writes=[sst])
            op("dve", lambda e: e.reciprocal(out=sst[:, col:col + 1], in_=sst[:, col:col + 1]), reads=[sst], writes=[sst])

        for it in range(nit):
            t0 = it * TC
            for tt in range(2):
                for b2 in range(2):
                    dma("sp", htok[tt], htok[tt][64 * b2:64 * b2 + 64, :], hsc[2 * tt + b2, t0:t0 + TC, :])
            for tt in range(2):
                op("dve", lambda e, tt=tt: e.scalar_tensor_tensor(out=tokA[:], in0=htok[tt][:], scalar=1.0, in1=htok[tt][:], op0=ALU.mult, op1=ALU.mult, accum_out=sst[:, tt:tt + 1]), reads=[htok[tt]], writes=[tokA, sst])
                rstd_from_ss2(tt)
                op("dve", lambda e, tt=tt: e.tensor_scalar(out=tokA[:], in0=htok[tt][:], scalar1=sst[:, tt:tt + 1], scalar2=None, op0=ALU.mult), reads=[htok[tt], sst], writes=[tokA])
                for kh in range(2):
                    pb = bank()
                    for k4 in range(4):
                        k = kh * 4 + k4
                        op("pe", lambda e, pb=pb, k=k, k4=k4: e.transpose(pb[:, k4 * 128:(k4 + 1) * 128], tokA[:, k * 128:(k + 1) * 128], ident), reads=[tokA, PB], writes=[pb])
                    gpre = P2("gpremlp", kh * 4, kh * 4 + 4).unsqueeze(2).to_broadcast([128, 4, 128])
                    op("dve", lambda e, pb=pb, kh=kh, tt=tt, gpre=gpre: e.tensor_tensor(out=hnT[:, kh * 4:kh * 4 + 4, tt * 128:(tt + 1) * 128], in0=pb[:, :].rearrange("p (k c) -> p k c", k=4), in1=gpre, op=ALU.mult), reads=[pb, PB], writes=[hnT])
            for ht in range(32):
                pb = bank()
                for k in range(8):
                    op("pe", lambda e, pb=pb, k=k, ht=ht: e.matmul(pb[:, 0:NCOL], lhsT=Wup[:, k, ht * 128:(ht + 1) * 128], rhs=hnT[:, k, :], start=(k == 0), stop=(k == 7)), reads=[Wup, hnT], writes=[pb])
                rr = rl[ht % 2]
                op("act", lambda e, pb=pb, rr=rr: e.activation(out=rr[:], in_=pb[:, 0:NCOL], func=AF.Relu), reads=[pb], writes=[rr])
                op("pool", lambda e, rr=rr, ht=ht: e.tensor_tensor(out=hid[:, ht, :], in0=rr[:], in1=rr[:], op=ALU.mult), reads=[rr], writes=[hid])
            for ct in range(8):
                pb = bank()
                for k in range(32):
                    op("pe", lambda e, pb=pb, k=k, ct=ct: e.matmul(pb[:, 0:NCOL], lhsT=Wdn[:, k, ct * 128:(ct + 1) * 128], rhs=hid[:, k, :], start=(k == 0), stop=(k == 31)), reads=[Wdn, hid], writes=[pb])
                op("act", lambda e, pb=pb, ct=ct: e.activation(out=ffT[:, ct, :], in_=pb[:, 0:NCOL], func=AF.Copy), reads=[pb], writes=[ffT])
            for tt in range(2):
                for kh in range(2):
                    pb = bank()
                    for k4 in range(4):
                        k = kh * 4 + k4
                        op("pe", lambda e, pb=pb, k=k, k4=k4, tt=tt: e.transpose(pb[:, k4 * 128:(k4 + 1) * 128], ffT[:, k, tt * 128:(tt + 1) * 128], ident), reads=[ffT, PB], writes=[pb])
                    op("dve", lambda e, pb=pb, kh=kh: e.tensor_copy(out=tokB[:, kh * 512:(kh + 1) * 512], in_=pb[:, :]), reads=[pb], writes=[tokB])
                op("dve", lambda e, tt=tt: e.scalar_tensor_tensor(out=tokA[:], in0=tokB[:], scalar=1.0, in1=tokB[:], op0=ALU.mult, op1=ALU.mult, accum_out=sst[:, 2 + tt:3 + tt]), reads=[tokB], writes=[tokA, sst])
                rstd_from_ss2(2 + tt)
                op("dve", lambda e, tt=tt: e.scalar_tensor_tensor(out=tokA[:], in0=tokB[:], scalar=sst[:, 2 + tt:3 + tt], in1=P2("gBmlp"), op0=ALU.mult, op1=ALU.mult), reads=[tokB, sst, PB], writes=[tokA])
                op("dve", lambda e, tt=tt: e.tensor_tensor(out=tokA[:], in0=tokA[:], in1=htok[tt][:], op=ALU.add), reads=[tokA, htok[tt]], writes=[tokA])
                for b2 in range(2):
                    dma("sp", None, out[2 * tt + b2, t0:t0 + TC, :], tokA[64 * b2:64 * b2 + 64, :], reads=[tokA], out_final=True)
        fw2.emit()
    fw.stack.close()
    return nc


_CACHE = {}


def kernel(**inputs):
    inp = {k: np.asarray(v) for k, v in inputs.items()}
    A, B = build_params(inp)
    ppa, ppb = A.pack(), B.pack()
    key = "full"
    if key not in _CACHE:
        _CACHE[key] = build_program(A.off, A.n, B.off, B.n)
    nc = _CACHE[key]
    x = np.ascontiguousarray(inp["x"], dtype=np.float32)
    sq = lambda k: np.ascontiguousarray(inp[k][0], dtype=np.float32)
    shared = {"ppa": ppa, "ppb": ppb, "w_in": sq("w_in"), "w_out": sq("w_out"), "w_glu": sq("s5_w_glu"),
              "w_ff_up": sq("w_ff_up"), "w_ff_down": sq("w_ff_down")}
    in_maps = [dict(shared, x=np.ascontiguousarray(x[c * NB:(c + 1) * NB])) for c in range(NCORES)]
    res = run_bass_kernel_spmd(nc, in_maps, core_ids=list(range(NCORES)))
    return np.concatenate([r["out"] for r in res.results], axis=0)
```
